# Optimizing a Trainium2 kernel written in Bass

```python
import math
import jax, jax.numpy as jnp
from jax import lax
import numpy as np

D_MODEL = 1024
BATCH = 16
SEQ = 2048
DEPTH = 2

SSM_WIDTH = D_MODEL // 4
SSM_GROUP = 16
SSM_GROUPS = SSM_WIDTH // SSM_GROUP
SSM_STATE = 64
RET_HEAD_DIM = 64
RET_WIDTH = D_MODEL // 4
RET_HEADS = RET_WIDTH // RET_HEAD_DIM
RET_CHUNK = 128
ATT_HEAD_DIM = 64
ATT_WIDTH = D_MODEL // 2
ATT_HEADS = ATT_WIDTH // ATT_HEAD_DIM
DILATED_BRANCHES = ((128, 1), (512, 4), (2048, 16))
ATT_BLOCK = 128
MIX_WIDTH = SSM_WIDTH + RET_WIDTH + ATT_WIDTH
IN_WIDTH = SSM_WIDTH + 4 * RET_WIDTH + 3 * ATT_WIDTH
D_FF = 4 * D_MODEL
DEEPNORM_ALPHA = (2 * DEPTH) ** 0.25
DEEPNORM_BETA = (8 * DEPTH) ** -0.25
LN_EPS = 1e-5
DT_MIN = 1e-3
DT_MAX = 1e-1

kernel_name = "hymba_s5_retnet_longnet_deepnorm"


def layer_norm(x, w, b):
    xf = x.astype(jnp.float32)
    mu = jnp.mean(xf, axis=-1, keepdims=True)
    var = jnp.mean(jnp.square(xf - mu), axis=-1, keepdims=True)
    y = (xf - mu) * lax.rsqrt(var + LN_EPS)
    return (y * w.astype(jnp.float32) + b.astype(jnp.float32)).astype(x.dtype)


def rms_norm(x, w):
    xf = x.astype(jnp.float32)
    y = xf * lax.rsqrt(jnp.mean(jnp.square(xf), axis=-1, keepdims=True) + LN_EPS)
    return (y * w.astype(jnp.float32)).astype(x.dtype)


def cmul(ar, ai, br, bi):
    return ar * br - ai * bi, ar * bi + ai * br


def s5_mixer(u, lam_re, lam_im, b_re, b_im, c_re, c_im, d_skip, log_dt, w_glu, b_glu):
    f32 = jnp.float32
    bsz, s, _ = u.shape
    uf = u.astype(f32).reshape(bsz, s, SSM_GROUPS, SSM_GROUP)
    lr, li = lam_re.astype(f32), lam_im.astype(f32)
    dt = jnp.exp(log_dt.astype(f32))[:, None]
    mag = jnp.exp(lr * dt)
    a_re, a_im = mag * jnp.cos(li * dt), mag * jnp.sin(li * dt)
    den = lr * lr + li * li
    nr, ni = a_re - 1.0, a_im
    f_re = (nr * lr + ni * li) / den
    f_im = (ni * lr - nr * li) / den
    br, bi = b_re.astype(f32), b_im.astype(f32)
    bb_re, bb_im = cmul(f_re[..., None], f_im[..., None], br, bi)
    bu_re = jnp.einsum('bsgh,gph->sbgp', uf, bb_re)
    bu_im = jnp.einsum('bsgh,gph->sbgp', uf, bb_im)
    a_re_t = jnp.broadcast_to(a_re[None, None], (s, 1, SSM_GROUPS, SSM_STATE))
    a_im_t = jnp.broadcast_to(a_im[None, None], (s, 1, SSM_GROUPS, SSM_STATE))

    def combine(e1, e2):
        a1r, a1i, b1r, b1i = e1
        a2r, a2i, b2r, b2i = e2
        ar, ai = cmul(a2r, a2i, a1r, a1i)
        xr, xi = cmul(a2r, a2i, b1r, b1i)
        return ar, ai, xr + b2r, xi + b2i

    _, _, st_re, st_im = lax.associative_scan(combine, (a_re_t, a_im_t, bu_re, bu_im), axis=0)
    y = (jnp.einsum('sbgp,ghp->bsgh', st_re, c_re.astype(f32))
         - jnp.einsum('sbgp,ghp->bsgh', st_im, c_im.astype(f32)))
    y = (y + d_skip.astype(f32).reshape(SSM_GROUPS, SSM_GROUP) * uf).reshape(bsz, s, SSM_WIDTH)
    g = jax.nn.gelu(y)
    out = g * jax.nn.sigmoid(g @ w_glu.astype(f32) + b_glu.astype(f32))
    return out.astype(u.dtype)


def retention_mixer(q, k, v, g, norm_w):
    f32 = jnp.float32
    bsz, s, h, dh = q.shape
    c = RET_CHUNK
    n = s // c
    lg = jnp.log(1.0 - 2.0 ** (-5.0 - jnp.arange(h, dtype=f32)))
    qf = q.astype(f32).reshape(bsz, n, c, h, dh)
    kf = (k.astype(f32) * (dh ** -0.5)).reshape(bsz, n, c, h, dh)
    vf = v.astype(f32).reshape(bsz, n, c, h, dh)
    idx = jnp.arange(c, dtype=f32)
    diff = idx[:, None] - idx[None, :]
    causal = diff >= 0
    dmat = jnp.where(causal[None], jnp.exp(jnp.where(causal, diff, 0.0)[None] * lg[:, None, None]), 0.0)
    scores = jnp.einsum('bnqhd,bnkhd->bnhqk', qf, kf) * dmat
    inner = jnp.einsum('bnhqk,bnkhe->bnqhe', scores, vf)
    zeta = jnp.exp((c - 1.0 - idx)[:, None] * lg[None, :])
    kv = jnp.einsum('bnkhd,bnkhe->nbhde', kf * zeta[None, None, :, :, None], vf)
    g_chunk = jnp.exp(c * lg)[None, :, None, None]

    def step(r, kv_n):
        return g_chunk * r + kv_n, r

    _, r_prev = lax.scan(step, jnp.zeros((bsz, h, dh, dh), f32), kv)
    xi = jnp.exp((idx + 1.0)[:, None] * lg[None, :])
    cross = jnp.einsum('bnqhd,nbhde->bnqhe', qf * xi[None, None, :, :, None], r_prev)
    o = (inner + cross).reshape(bsz, s, h, dh)
    mu = jnp.mean(o, axis=-1, keepdims=True)
    var = jnp.mean(jnp.square(o - mu), axis=-1, keepdims=True)
    o = ((o - mu) * lax.rsqrt(var + LN_EPS)).reshape(bsz, s, h * dh) * norm_w.astype(f32)
    return (jax.nn.silu(g.astype(f32)) * o).astype(q.dtype)


def alibi_slopes(n_heads):
    return 2.0 ** (-8.0 * jnp.arange(1, n_heads + 1, dtype=jnp.float32) / n_heads)


def dilated_branch(q, k, v, window, dilation, slopes):
    f32 = jnp.float32
    bsz, s, h, dh = q.shape
    sub_len = s // dilation
    wsub = window // dilation
    qb = ATT_BLOCK
    nb = -(-sub_len // qb)
    lp = nb * qb

    def to_sub(t):
        t = t.reshape(bsz, sub_len, dilation, h, dh).transpose(0, 2, 3, 1, 4)
        t = jnp.pad(t, ((0, 0), (0, 0), (0, 0), (0, lp - sub_len), (0, 0)))
        return t.reshape(bsz, dilation, h, nb, qb, dh)

    def with_prev(t):
        prev = jnp.concatenate([jnp.zeros_like(t[:, :, :, :1]), t[:, :, :, :-1]], axis=3)
        return jnp.concatenate([prev, t], axis=4)

    qs = to_sub(q)
    ks = with_prev(to_sub(k))
    vs = with_prev(to_sub(v))
    sc = jnp.einsum('brhnqc,brhnkc->brhnqk', qs, ks).astype(f32) * (dh ** -0.5)
    qi = jnp.arange(qb)
    ki = jnp.arange(2 * qb)
    dist = (qb + qi)[:, None] - ki[None, :]
    key_pos = jnp.arange(nb)[:, None] * qb + ki[None, :] - qb
    valid = ((dist >= 0) & (dist <= wsub))[None] & (key_pos >= 0)[:, None, :]
    bias = -slopes[:, None, None, None] * (dilation * dist).astype(f32)[None, None]
    sc = jnp.where(valid, sc + bias, -jnp.inf)
    m = jnp.max(sc, axis=-1, keepdims=True)
    p = jnp.exp(sc - m)
    l = jnp.sum(p, axis=-1)
    o = jnp.einsum('brhnqk,brhnkc->brhnqc', p, vs.astype(f32)) / l[..., None]
    lse = m[..., 0] + jnp.log(l)
    o = o.reshape(bsz, dilation, h, lp, dh)[:, :, :, :sub_len].transpose(0, 3, 1, 2, 4).reshape(bsz, s, h, dh)
    lse = lse.reshape(bsz, dilation, h, lp)[:, :, :, :sub_len].transpose(0, 3, 1, 2).reshape(bsz, s, h)
    return o, lse


def dilated_attention_mixer(q, k, v):
    bsz, s, h, dh = q.shape
    slopes = alibi_slopes(h)
    outs, lses = [], []
    for window, dilation in DILATED_BRANCHES:
        o, lse = dilated_branch(q, k, v, window, dilation, slopes)
        outs.append(o)
        lses.append(lse)
    wts = jax.nn.softmax(jnp.stack(lses, axis=-1), axis=-1)
    o = sum(wts[..., i, None] * outs[i] for i in range(len(outs)))
    return o.reshape(bsz, s, h * dh).astype(q.dtype)


def hybrid_mixer(x, w_in, lam_re, lam_im, b_re, b_im, c_re, c_im, d_skip, log_dt, w_glu, b_glu,
                 ssm_norm, ret_norm, attn_norm, w_out):
    bsz, s, _ = x.shape
    z = x @ w_in
    cuts = np.cumsum([SSM_WIDTH] + [RET_WIDTH] * 4 + [ATT_WIDTH] * 2).tolist()
    u, rq, rk, rv, rg, aq, ak, av = jnp.split(z, cuts, axis=-1)
    y_ssm = rms_norm(s5_mixer(u, lam_re, lam_im, b_re, b_im, c_re, c_im, d_skip, log_dt, w_glu, b_glu), ssm_norm)
    rh = lambda t: t.reshape(bsz, s, RET_HEADS, RET_HEAD_DIM)
    y_ret = retention_mixer(rh(rq), rh(rk), rh(rv), rg, ret_norm)
    ah = lambda t: t.reshape(bsz, s, ATT_HEADS, ATT_HEAD_DIM)
    y_att = rms_norm(dilated_attention_mixer(ah(aq), ah(ak), ah(av)), attn_norm)
    return jnp.concatenate([y_ssm, y_ret, y_att], axis=-1) @ w_out


def setup_inputs(seed: int = 0) -> dict:
    key = jax.random.key(seed)
    ks = jax.random.split(key, 24)
    f32 = jnp.float32
    nrm = lambda k, shp, sc: jax.random.normal(k, shp, f32) * sc
    L, G, P, H = DEPTH, SSM_GROUPS, SSM_STATE, SSM_GROUP
    lam_im = jnp.broadcast_to(math.pi * jnp.arange(P, dtype=f32), (L, G, P)) + nrm(ks[3], (L, G, P), 0.01)
    return {
        "x": nrm(ks[0], (BATCH, SEQ, D_MODEL), 1.0),
        "w_in": nrm(ks[1], (L, D_MODEL, IN_WIDTH), D_MODEL ** -0.5),
        "ssm_lambda_re": -0.5 + nrm(ks[2], (L, G, P), 0.01),
        "ssm_lambda_im": lam_im,
        "ssm_b_re": nrm(ks[4], (L, G, P, H), (2 * H) ** -0.5),
        "ssm_b_im": nrm(ks[5], (L, G, P, H), (2 * H) ** -0.5),
        "ssm_c_re": nrm(ks[6], (L, G, H, P), (2 * P) ** -0.5),
        "ssm_c_im": nrm(ks[7], (L, G, H, P), (2 * P) ** -0.5),
        "ssm_d": nrm(ks[8], (L, SSM_WIDTH), 1.0),
        "ssm_log_dt": jax.random.uniform(ks[9], (L, G), f32, math.log(DT_MIN), math.log(DT_MAX)),
        "ssm_w_glu": nrm(ks[10], (L, SSM_WIDTH, SSM_WIDTH), SSM_WIDTH ** -0.5),
        "ssm_b_glu": nrm(ks[11], (L, SSM_WIDTH), 0.01),
        "ssm_out_norm": 1.0 + nrm(ks[12], (L, SSM_WIDTH), 0.02),
        "ret_out_norm": 1.0 + nrm(ks[13], (L, RET_WIDTH), 0.02),
        "attn_out_norm": 1.0 + nrm(ks[14], (L, ATT_WIDTH), 0.02),
        "w_out": nrm(ks[15], (L, MIX_WIDTH, D_MODEL), MIX_WIDTH ** -0.5 * DEEPNORM_BETA),
        "ln1_w": 1.0 + nrm(ks[16], (L, D_MODEL), 0.02),
        "ln1_b": nrm(ks[17], (L, D_MODEL), 0.02),
        "mlp_w1": nrm(ks[18], (L, D_MODEL, D_FF), D_MODEL ** -0.5),
        "mlp_w2": nrm(ks[19], (L, D_FF, D_MODEL), D_FF ** -0.5 * DEEPNORM_BETA),
        "ln2_w": 1.0 + nrm(ks[20], (L, D_MODEL), 0.02),
        "ln2_b": nrm(ks[21], (L, D_MODEL), 0.02),
    }


def reference(x, w_in, ssm_lambda_re, ssm_lambda_im, ssm_b_re, ssm_b_im, ssm_c_re, ssm_c_im,
              ssm_d, ssm_log_dt, ssm_w_glu, ssm_b_glu, ssm_out_norm, ret_out_norm, attn_out_norm,
              w_out, ln1_w, ln1_b, mlp_w1, mlp_w2, ln2_w, ln2_b):
    for i in range(DEPTH):
        h = hybrid_mixer(x, w_in[i], ssm_lambda_re[i], ssm_lambda_im[i], ssm_b_re[i], ssm_b_im[i],
                         ssm_c_re[i], ssm_c_im[i], ssm_d[i], ssm_log_dt[i], ssm_w_glu[i], ssm_b_glu[i],
                         ssm_out_norm[i], ret_out_norm[i], attn_out_norm[i], w_out[i])
        x = layer_norm(DEEPNORM_ALPHA * x + h, ln1_w[i], ln1_b[i])
        h = jnp.square(jax.nn.relu(x @ mlp_w1[i])) @ mlp_w2[i]
        x = layer_norm(DEEPNORM_ALPHA * x + h, ln2_w[i], ln2_b[i])
    return x
```

```python
import math
from contextlib import ExitStack
import numpy as np
import ml_dtypes
import concourse.bass as bass
import concourse.mybir as mybir
from concourse.bass_utils import run_bass_kernel_spmd

F32 = mybir.dt.float32
BF16 = mybir.dt.bfloat16
AF = mybir.ActivationFunctionType
ALU = mybir.AluOpType

D = 1024
SEQ = 2048
NSEQ = 2
T = 512
NT = SEQ // T
INW = 2816
DFF = 4096
DEPTH = 2
ALPHA = (2 * DEPTH) ** 0.25
EPS = 1e-5
NW = 3
OUTROT_ENG = "pool"


class Op:
    __slots__ = ("eng", "fn", "deps", "dma", "need_inc", "cnt", "dsem", "dval", "idx")


class Prog:
    COMPUTE = ("pe", "act", "dve", "pool")

    def __init__(self):
        self.ops = []
        self.res = {}

    def add(self, eng, fn, reads=(), writes=(), dma=False):
        idx = len(self.ops)
        deps = set()
        for k in reads:
            st = self.res.get(k)
            if st is None:
                st = self.res[k] = [None, {}, []]
            if st[0] is not None:
                deps.add(st[0])
        for k in writes:
            st = self.res.get(k)
            if st is None:
                st = self.res[k] = [None, {}, []]
            if st[0] is not None:
                deps.add(st[0])
            deps.update(st[1].values())
            deps.update(st[2])
        for k in reads:
            st = self.res[k]
            if dma:
                st[2].append(idx)
            else:
                st[1][eng] = idx
        for k in writes:
            self.res[k] = [idx, {}, []]
        deps.discard(idx)
        o = Op()
        o.eng, o.fn, o.deps, o.dma, o.idx = eng, fn, deps, dma, idx
        o.need_inc = False
        o.cnt = 0
        o.dsem = None
        o.dval = 0
        self.ops.append(o)
        return idx

    def emit(self, nc, stack, n_dma_sems=None):
        n_dma_sems = n_dma_sems or {"sp": 14, "pool": 8, "act": 4}
        ops = self.ops
        for o in ops:
            for p in o.deps:
                P = ops[p]
                if P.dma:
                    continue
                if P.eng == "pe" and o.eng == "pe" and not o.dma:
                    continue
                P.need_inc = True
        engsem = {e: stack.enter_context(nc.semaphore("s_" + e)) for e in self.COMPUTE}
        cnt = {e: 0 for e in self.COMPUTE}
        dsems = {}
        dstate = {}
        for q, n in n_dma_sems.items():
            dsems[q] = [stack.enter_context(nc.semaphore("d_%s%d" % (q, i))) for i in range(n)]
            dstate[q] = [0, [0] * n]
        for o in ops:
            if o.dma:
                st = dstate[o.eng]
                i = st[0] % len(dsems[o.eng])
                st[0] += 1
                st[1][i] += 16
                o.dsem = dsems[o.eng][i]
                o.dval = st[1][i]
            elif o.need_inc:
                cnt[o.eng] += 1
                o.cnt = cnt[o.eng]
        byeng = {e: [] for e in ("pe", "act", "dve", "pool", "sp")}
        for o in ops:
            byeng[o.eng].append(o)
        final_waits = []
        for q in dsems:
            for i, s in enumerate(dsems[q]):
                if dstate[q][1][i] > 0:
                    final_waits.append((s, dstate[q][1][i]))

        def run(engname, e):
            waited = {}
            for o in byeng[engname]:
                waits = {}
                for p in o.deps:
                    P = ops[p]
                    if P.dma:
                        sem, val = P.dsem, P.dval
                    else:
                        if P.eng == "pe" and engname == "pe" and not o.dma:
                            continue
                        sem, val = engsem[P.eng], P.cnt
                    key = id(sem)
                    if key not in waits or waits[key][1] < val:
                        waits[key] = (sem, val)
                if o.dma and o.dval > 16:
                    key = id(o.dsem)
                    v = o.dval - 16
                    if key not in waits or waits[key][1] < v:
                        waits[key] = (o.dsem, v)
                for key, (sem, val) in waits.items():
                    if waited.get(key, 0) < val:
                        e.wait_ge(sem, val)
                        waited[key] = val
                ins = o.fn(e)
                if o.dma:
                    ins.then_inc(o.dsem, 16)
                elif o.need_inc:
                    ins.then_inc(engsem[engname], 1)
            if engname == "sp":
                for sem, val in final_waits:
                    e.wait_ge(sem, val)

        with nc.Block() as block:
            @block.tensor
            def _(e):
                run("pe", e)

            @block.scalar
            def _(e):
                run("act", e)

            @block.vector
            def _(e):
                run("dve", e)

            @block.gpsimd
            def _(e):
                run("pool", e)

            @block.sync
            def _(e):
                run("sp", e)


def make_consts():
    bf = ml_dtypes.bfloat16
    c = {}
    c["ident"] = np.eye(128, dtype=np.float32).astype(bf)
    c["ones"] = np.ones((128, 128), np.float32).astype(bf)
    blk = np.zeros((128, 128), np.float32)
    blk[:64, :64] = 1.0 / 64
    blk[64:, 64:] = 1.0 / 64
    c["blk64"] = blk.astype(bf)
    slopes = 2.0 ** (-8.0 * np.arange(1, 9, dtype=np.float64) / 8)
    k = np.arange(128, dtype=np.float64)[:, None]
    q = np.arange(128, dtype=np.float64)[None, :]

    def dtab(dil):
        t = np.zeros((128, 8, 2, 128), np.float64)
        for h in range(8):
            dc = q - k
            t[:, h, 0, :] = np.where(dc >= 0, np.exp(-slopes[h] * dil * np.maximum(dc, 0)), 0.0)
            dp = 128 + q - k
            t[:, h, 1, :] = np.where(dp <= 128, np.exp(-slopes[h] * dil * dp), 0.0)
        return t.reshape(128, 4, 2, 2, 128).astype(np.float32).astype(bf)

    c["d1"] = dtab(1)
    c["d2"] = dtab(4)
    d3 = np.zeros((128, 8, 128), np.float64)
    for h in range(8):
        dc = q - k
        d3[:, h, :] = np.where(dc >= 0, np.exp(-slopes[h] * 16 * np.maximum(dc, 0)), 0.0)
    c["d3"] = d3.reshape(128, 4, 2, 128).astype(np.float32).astype(bf)
    gam = 1.0 - 2.0 ** (-5.0 - np.arange(4, dtype=np.float64))
    dr = np.zeros((128, 2, 2, 128), np.float64)
    for h in range(4):
        dc = q - k
        dr[:, h % 2, h // 2, :] = np.where(dc >= 0, gam[h] ** np.maximum(dc, 0), 0.0) / 8.0
    c["dr"] = dr.astype(np.float32).astype(bf)
    zeta = np.zeros((128, 4), np.float64)
    for h in range(4):
        zeta[:, h] = gam[h] ** (127 - np.arange(128)) / 8.0
    c["zeta"] = zeta.astype(np.float32)
    xi = np.zeros((128, 2, 128), np.float64)
    gch = np.zeros((128, 2), np.float64)
    for h in range(4):
        hp, h2 = h // 2, h % 2
        xi[64 * h2:64 * h2 + 64, hp, :] = (gam[h] ** (np.arange(128) + 1.0))[None, :]
        gch[64 * h2:64 * h2 + 64, hp] = gam[h] ** 128
    c["xi"] = xi.astype(np.float32).astype(bf)
    c["gch"] = gch.astype(np.float32)
    return c


CONST_SPECS = [
    ("ident", [128, 128], BF16), ("ones", [128, 128], BF16), ("blk64", [128, 128], BF16),
    ("d1", [128, 4, 2, 2, 128], BF16), ("d2", [128, 4, 2, 2, 128], BF16),
    ("d3", [128, 4, 2, 128], BF16), ("dr", [128, 2, 2, 128], BF16),
    ("zeta", [128, 4], F32), ("xi", [128, 2, 128], BF16), ("gch", [128, 2], F32),
]

PARAM_SPECS = [
    ("bt", [DEPTH, 128, 8, 2, 128], F32),
    ("ct", [DEPTH, 128, 8, 2, 128], F32),
    ("lam", [DEPTH, 128, 3, 8], F32),
    ("vecs", [DEPTH, 128, 12], F32),
    ("lnp", [DEPTH, 4, 128, 1024], F32),
]


def layout_params(inp):
    L = DEPTH
    bt = np.zeros((L, 128, 8, 2, 128), np.float32)
    ct = np.zeros((L, 128, 8, 2, 128), np.float32)
    for g in range(16):
        gp, g2, g8 = g // 2, g % 2, g % 8
        for ri, (bname, cname) in enumerate((("ssm_b_re", "ssm_c_re"), ("ssm_b_im", "ssm_c_im"))):
            b = inp[bname][:, g]
            bt[:, g8 * 16:(g8 + 1) * 16, gp, ri, g2 * 64:(g2 + 1) * 64] = np.transpose(b, (0, 2, 1))
            cc = inp[cname][:, g]
            ct[:, g2 * 64:(g2 + 1) * 64, gp, ri, g8 * 16:(g8 + 1) * 16] = np.transpose(cc, (0, 2, 1))
    lam = np.zeros((L, 128, 3, 8), np.float32)
    for g in range(16):
        gp, g2 = g // 2, g % 2
        lam[:, g2 * 64:(g2 + 1) * 64, 0, gp] = inp["ssm_lambda_re"][:, g]
        lam[:, g2 * 64:(g2 + 1) * 64, 1, gp] = inp["ssm_lambda_im"][:, g]
        lam[:, g2 * 64:(g2 + 1) * 64, 2, gp] = inp["ssm_log_dt"][:, g][:, None]
    vecs = np.zeros((L, 128, 12), np.float32)
    vecs[:, :, 0:2] = inp["ssm_d"].reshape(L, 2, 128).transpose(0, 2, 1)
    vecs[:, :, 2:4] = inp["ssm_b_glu"].reshape(L, 2, 128).transpose(0, 2, 1)
    vecs[:, :, 4:6] = inp["ssm_out_norm"].reshape(L, 2, 128).transpose(0, 2, 1)
    vecs[:, :, 6:8] = inp["ret_out_norm"].reshape(L, 2, 128).transpose(0, 2, 1)
    vecs[:, :, 8:12] = inp["attn_out_norm"].reshape(L, 4, 128).transpose(0, 2, 1)
    lnp = np.stack([inp["ln1_w"], inp["ln1_b"], inp["ln2_w"], inp["ln2_b"]], axis=1)
    lnp = np.ascontiguousarray(np.broadcast_to(lnp[:, :, None, :], (L, 4, 128, 1024))).astype(np.float32)
    return {"bt": bt, "ct": ct, "lam": lam, "vecs": vecs, "lnp": lnp}


WEIGHT_SPECS = [
    ("w_in", [DEPTH, D, INW]), ("w_out", [DEPTH, D, D]), ("mlp_w1", [DEPTH, D, DFF]),
    ("mlp_w2", [DEPTH, DFF, D]), ("ssm_w_glu", [DEPTH, 256, 256]),
]


class Builder:
    def __init__(self, layers, nseq=NSEQ, ntiles=NT, taps=(), phases=None):
        self.layers = list(layers)
        self.nseq = nseq
        self.ntiles = ntiles
        self.taps = set(taps)
        self.phases = phases
        self.nc = bass.Bass("TRN2", target_bir_lowering=False)
        self.p = Prog()
        self.stack = ExitStack()
        self.tap_specs = {}
        self.psrot = 0
        self.wrot = 0
        self.dummy_n = 0
        self.held = set()
        self.lnrot = 0
        self.s5pp = 0

    def sb(self, name, shape, dt):
        return self.stack.enter_context(self.nc.sbuf_tensor("sb_" + name, list(shape), dt))

    def dram_in(self, name, shape, dt):
        return self.nc.dram_tensor(name, list(shape), dt, kind="ExternalInput").ap()

    def dram_out(self, name, shape, dt):
        return self.nc.dram_tensor(name, list(shape), dt, kind="ExternalOutput").ap()

    def mm(self, out, lhsT, rhs, start, stop, reads, writes, **kw):
        rb, cb = lhsT.base_partition(), out.base_partition()
        if (rb or cb) and "tile_position" not in kw:
            kw["tile_position"] = (rb, cb)
        self.p.add("pe", lambda e: e.matmul(out, lhsT=lhsT, rhs=rhs, start=start, stop=stop, **kw), reads, writes)

    def tr(self, out, in_, reads, writes):
        ident = self.ident[:]
        self.p.add("pe", lambda e: e.transpose(out=out, in_=in_, identity=ident), list(reads) + ["const"], writes)

    def act(self, out, in_, func, reads, writes, **kw):
        self.p.add("act", lambda e: e.activation(out=out, in_=in_, func=func, **kw), list(reads) + ["epsb_k"], writes)

    def tt(self, eng, out, in0, in1, op, reads, writes):
        self.p.add(eng, lambda e: e.tensor_tensor(out=out, in0=in0, in1=in1, op=op), reads, writes)

    def ts(self, eng, out, in0, s1, s2, op0, op1, reads, writes):
        if op1 is None:
            self.p.add(eng, lambda e: e.tensor_scalar(out=out, in0=in0, scalar1=s1, scalar2=None, op0=op0), reads, writes)
        else:
            self.p.add(eng, lambda e: e.tensor_scalar(out=out, in0=in0, scalar1=s1, scalar2=s2, op0=op0, op1=op1), reads, writes)

    def stt(self, out, in0, scalar, in1, op0, op1, reads, writes):
        self.p.add("dve", lambda e: e.scalar_tensor_tensor(out=out, in0=in0, scalar=scalar, in1=in1, op0=op0, op1=op1), reads, writes)

    def cp(self, eng, out, in_, reads, writes):
        if eng == "act":
            self.act(out, in_, AF.Copy, reads, writes)
        else:
            self.p.add(eng, lambda e: e.tensor_copy(out=out, in_=in_), reads, writes)

    def recip_act(self, out, in_, reads, writes):
        self.act(out, in_, AF.Ln, reads, writes)
        self.act(out, out, AF.Exp, writes, writes, scale=-1.0)

    def rsqrt_act(self, out, in_, scale, reads, writes):
        self.act(out, in_, AF.Ln, reads, writes, scale=scale, bias=self.epsb[:, 0:1])
        self.act(out, out, AF.Exp, writes, writes, scale=-0.5)

    def recip(self, out, in_, reads, writes):
        self.p.add("dve", lambda e: e.reciprocal(out=out, in_=in_), reads, writes)

    def scan(self, out, d0, d1, init, reads, writes):
        self.p.add("dve", lambda e: e.tensor_tensor_scan(out=out, data0=d0, data1=d1, initial=init, op0=ALU.mult, op1=ALU.add), reads, writes)

    def memset(self, eng, ap, val, reads, writes):
        self.p.add(eng, lambda e: e.memset(ap, val), reads, writes)

    def dma(self, q, out, in_, reads, writes):
        self.p.add(q, lambda e: e.dma_start(out=out, in_=in_), reads, writes, dma=True)

    def join(self, key_write, extra_reads=()):
        d = self.dummy
        self.p.add("pool", lambda e: e.memset(d[:], 0.0), list(extra_reads) + ["dummy_r"], [key_write, "dummy"])

    def bank(self, hold=False):
        while True:
            i = self.psrot % 8
            self.psrot += 1
            if i not in self.held:
                break
        if hold:
            self.held.add(i)
        return self.ps[i], ("ps", i)

    def release(self, *keys):
        for k in keys:
            self.held.discard(k[1])

    def tap(self, name, ap, shape, dt, reads):
        if name not in self.taps:
            return
        if "tap_" + name not in self.tap_specs:
            self.tap_specs["tap_" + name] = self.dram_out("tap_" + name, shape, dt)
        t = self.tap_specs["tap_" + name]
        self.dma("sp", t, ap, reads, ["tapout_" + name])

    def wplan_build(self):
        self.wplan = []
        for li, l in enumerate(self.layers):
            win, wo, w1, w2 = self.w["w_in"][l], self.w["w_out"][l], self.w["mlp_w1"][l], self.w["mlp_w2"][l]
            for s in range(self.nseq):
                for n in range(self.ntiles):
                    ft = (s == 0 and n == 0)
                    for b in range(6):
                        ncols = 512 if b < 5 else 256
                        self.wplan.append((win[:, b * 512:b * 512 + ncols], b, ncols, ft))
                    for h in range(2):
                        self.wplan.append((wo[:, h * 512:(h + 1) * 512], 6 + h, 512, ft))
                    for b in range(8):
                        self.wplan.append((w1[:, b * 512:(b + 1) * 512], 8 + b, 512, ft))
                    for h in range(2):
                        for q in range(4):
                            self.wplan.append((w2[q * 1024:(q + 1) * 1024, h * 512:(h + 1) * 512], 16 + 4 * h + q, 512, ft))
        self.wptr = 0
        self.wissued = 0

    def _wissue(self, i):
        src_ap, blk, ncols, ft = self.wplan[i]
        buf = self.wpool[i % NW]
        key = ("w", i % NW)
        scr = self.wscr[blk][:, 0:8 * ncols].rearrange("p (k c) -> p k c", c=ncols)
        if ft:
            self.dma("pool", buf[:, :, 0:ncols], src_ap.rearrange("(kt p) c -> p kt c", p=128), [], [key])
            self.dma("sp", scr, buf[:, :, 0:ncols], [key], [("wscr", blk)])
        else:
            self.dma("pool", buf[:, :, 0:ncols], scr, [("wscr", blk)], [key])

    def wblock(self, blk, look=2):
        i = self.wptr
        assert self.wplan[i][1] == blk, (i, self.wplan[i][1], blk)
        lim = min(len(self.wplan), i + look + 1)
        while self.wissued < lim:
            self._wissue(self.wissued)
            self.wissued += 1
        self.wptr += 1
        return self.wpool[i % NW], ("w", i % NW)

    def build(self):
        nc = self.nc
        nseq, ntiles = self.nseq, self.ntiles
        ntok = nseq * SEQ
        self.x_in = self.dram_in("x", [ntok, D], F32)
        self.out = self.dram_out("out", [ntok, D], F32)
        self.w = {}
        for name, shape in WEIGHT_SPECS:
            self.w[name] = self.dram_in(name, shape, F32)
        self.prm = {}
        for name, shape, dt in PARAM_SPECS:
            self.prm[name] = self.dram_in(name, shape, dt)
        self.cst_d = {}
        for name, shape, dt in CONST_SPECS:
            self.cst_d[name] = self.dram_in("c_" + name, shape, dt)
        self.xmid = nc.dram_tensor("xmid", [ntok, D], F32).ap()
        self.vscr = [nc.dram_tensor("vscr%d" % i, [T, 512], BF16).ap() for i in range(2)]
        self.wscr = nc.dram_tensor("wscr", [24, 128, 4096], BF16).ap()

        sb = self.sb
        self.cst = {}
        for name, shape, dt in CONST_SPECS:
            self.cst[name] = sb("k_" + name, shape, dt)
        self.ident = self.cst["ident"]
        self.dummy = sb("dmy0", [128, 8], F32)
        self.xtok = sb("xtok", [128, 4, D], F32)
        self.xT = sb("xT", [128, 8, T], BF16)
        self.khist = sb("khist", [128, 4, SEQ], BF16)
        self.v3 = sb("v3", [128, 16, 512], BF16)
        self.v1 = sb("v1", [128, 5, 512], BF16)
        self.v4 = sb("v4", [128, 2, 4, 512], BF16)
        self.wpool = [sb("wp%d" % i, [128, 8, 512], BF16) for i in range(NW)]
        self.lnp = sb("lnp", [128, 2, D], F32)
        self.btb = sb("btb", [128, 8, 2, 128], BF16)
        self.ctb = sb("ctb", [128, 8, 2, 128], BF16)
        self.tabc = sb("tabc", [128, 8, 128], F32)
        self.tabs = sb("tabs", [128, 8, 2, 128], F32)
        self.s5p = sb("s5p", [128, 20, 8], F32)
        self.vecs = sb("vecs", [128, 12], F32)
        self.wglu = sb("wglu", [128, 2, 256], BF16)
        self.diagd = sb("diagd", [128, 2, 128], BF16)
        self.carry = sb("carry", [128, 2, 8], F32)
        self.wl = sb("wl", [128, 2, 8], F32)
        self.r32 = sb("r32", [128, 2, 64], F32)
        self.rbf = sb("rbf", [128, 2, 64], BF16)
        self.lns = sb("lns", [128, 2, 8], F32)
        self.bnst = sb("bnst", [128, 2, 2, 6], F32)
        self.regA = sb("regA", [128, 32 * T], BF16)
        rA = self.regA
        self.hidden = rA[:, :].rearrange("p (k t) -> p k t", t=T)
        self.zT = rA[:, 0:12 * T].rearrange("p (k t) -> p k t", t=T)
        o = 12 * T
        self.rkz = rA[:, o:o + 1024].rearrange("p (c d) -> p c d", c=4)
        o += 1024
        self.rvt = rA[:, o:o + 1024].rearrange("p (c d) -> p c d", c=4)
        o += 1024
        self.rqx = rA[:, o:o + 2 * T].rearrange("p (k t) -> p k t", t=T)
        o += 2 * T
        def f32view(n):
            nonlocal o
            v = rA[:, o:o + 2 * n].bitcast(F32)
            o += 2 * n
            return v
        self.s5t1 = [f32view(256) for _ in range(2)]
        self.s5t2 = [f32view(256) for _ in range(2)]
        self.s5w = [f32view(384) for _ in range(4)]
        self.s5ta = [f32view(256) for _ in range(2)]
        self.s5x = []
        for _ in range(4):
            self.s5x.append(rA[:, o:o + 256])
            o += 256
        assert o <= 32 * T, o
        self.yT = sb("yT", [128, 8, T], BF16)
        self.fs = [sb("fs%d" % i, [128, T], F32) for i in range(6)]
        self.bs = [sb("bs%d" % i, [128, T], BF16) for i in range(8)]
        self.xbn = sb("xbn", [128, 4, D], BF16)
        self.xb = sb("xb", [128, D], BF16)
        self.ps = [self.stack.enter_context(nc.psum_tensor("ps%d" % i, [128, 512], F32)) for i in range(8)]

        for name, shape, dt in CONST_SPECS:
            self.dma("sp", self.cst[name][:], self.cst_d[name], [], ["const"])
        self.memset("pool", self.dummy[:], 0.0, [], ["dummy"])

        self.wplan_build()
        self.tiles = [(li, l, s, n) for li, l in enumerate(self.layers) for s in range(nseq) for n in range(ntiles)]
        self.prefetch_x(0)
        for ti, (li, l, s, n) in enumerate(self.tiles):
            first = li == 0
            last = li == len(self.layers) - 1
            if s == 0 and n == 0:
                self.layer_setup(l)
            if n == 0:
                self.seq_reset()
            self.ti = ti
            self.tile(l, s, n, first, last)
        self.p.emit(nc, self.stack)
        return nc

    def layer_setup(self, l):
        P = self.prm
        s5p = self.s5p
        rk = ["s5prm"]
        self.dma("sp", s5p[:, 0:3, :], P["lam"][l], [], rk)
        self.dma("sp", self.vecs[:], P["vecs"][l], [], ["vecs"])
        self.dma("pool", self.btb[:], P["bt"][l], [], ["btb"])
        self.dma("pool", self.wglu[:], self.w["ssm_w_glu"][l].rearrange("(kt p) c -> p kt c", p=128), [], ["wglu"])
        ct_st = [self.fs[i] for i in range(4)]
        for i in range(4):
            self.dma("sp", ct_st[i][:, :].rearrange("p (g r c) -> p g r c", g=2, r=2),
                     P["ct"][l][:, 2 * i:2 * i + 2], [("fs", i)], [("fs", i)])
        lr, li_, ldt = s5p[:, 0, :], s5p[:, 1, :], s5p[:, 2, :]
        dt_, th, mag = s5p[:, 3, :], s5p[:, 4, :], s5p[:, 5, :]
        cc, ss = s5p[:, 6, :], s5p[:, 7, :]
        t1, t2, t3 = s5p[:, 8, :], s5p[:, 9, :], s5p[:, 10, :]
        fre, fim = s5p[:, 11, :], s5p[:, 12, :]
        are, aim = s5p[:, 13, :], s5p[:, 14, :]
        den = s5p[:, 15, :]
        A = lambda out, in_, f, **kw: self.act(out, in_, f, rk, rk, **kw)
        V = lambda out, a, b, op: self.tt("dve", out, a, b, op, rk, rk)
        A(dt_, ldt, AF.Exp)
        V(t1, lr, dt_, ALU.mult)
        A(mag, t1, AF.Exp)
        V(th, li_, dt_, ALU.mult)
        A(ss, th, AF.Sin, scale=1.0 / 16)
        self.ts("dve", t2, th, 1.0 / 16, math.pi / 2, ALU.mult, ALU.add, rk, rk)
        A(cc, t2, AF.Sin)
        for _ in range(4):
            V(t1, cc, cc, ALU.mult)
            V(t2, ss, ss, ALU.mult)
            V(t3, cc, ss, ALU.mult)
            V(cc, t1, t2, ALU.subtract)
            self.ts("dve", ss, t3, 2.0, None, ALU.mult, None, rk, rk)
        V(are, mag, cc, ALU.mult)
        V(aim, mag, ss, ALU.mult)
        V(t1, lr, lr, ALU.mult)
        V(t2, li_, li_, ALU.mult)
        V(den, t1, t2, ALU.add)
        self.recip(den, den, rk, rk)
        self.ts("dve", t3, are, -1.0, None, ALU.add, None, rk, rk)
        V(t1, t3, lr, ALU.mult)
        V(t2, aim, li_, ALU.mult)
        V(t1, t1, t2, ALU.add)
        V(fre, t1, den, ALU.mult)
        V(t1, aim, lr, ALU.mult)
        V(t2, t3, li_, ALU.mult)
        V(t1, t1, t2, ALU.subtract)
        V(fim, t1, den, ALU.mult)
        for gp in range(8):
            st = ct_st[gp // 2][:, :].rearrange("p (g r c) -> p g r c", g=2, r=2)
            cre, cim = st[:, gp % 2, 0, :], st[:, gp % 2, 1, :]
            fk = ("fs", gp // 2)
            tmpa = self.fs[4][:, 0:128]
            tmpb = self.fs[4][:, 128:256]
            tk = [("fs", 4)]
            self.ts("dve", tmpa, cre, fre[:, gp:gp + 1], None, ALU.mult, None, rk + [fk], tk)
            self.stt(tmpb, cim, fim[:, gp:gp + 1], tmpa, ALU.mult, ALU.subtract, rk + [fk] + tk, tk)
            self.ts("dve", self.ctb[:, gp, 0, :], tmpb, -1.0, None, ALU.mult, None, tk, ["ctb"])
            self.ts("dve", tmpa, cre, fim[:, gp:gp + 1], None, ALU.mult, None, rk + [fk], tk)
            self.stt(tmpb, cim, fre[:, gp:gp + 1], tmpa, ALU.mult, ALU.add, rk + [fk] + tk, tk)
            self.ts("dve", self.ctb[:, gp, 1, :], tmpb, -1.0, None, ALU.mult, None, tk, ["ctb"])
        tc, tsn = self.tabc, self.tabs
        tk = ["tab"]
        self.cp("dve", tc[:, :, 0], cc, rk, tk)
        self.cp("dve", tsn[:, :, 0, 0], ss, rk, tk)
        pc, psn = s5p[:, 8, :], s5p[:, 9, :]
        self.cp("dve", pc, cc, rk, rk)
        self.cp("dve", psn, ss, rk, rk)
        q1, q2, q3 = s5p[:, 10, :], s5p[:, 13, :], s5p[:, 14, :]
        Lc = 1
        tmpA = self.fs[4][:, :].rearrange("p (g j) -> p g j", g=8)
        tmpB = self.fs[5][:, :].rearrange("p (g j) -> p g j", g=8)
        while Lc < 128:
            pcb = pc.unsqueeze(2).to_broadcast([128, 8, Lc])
            psb = psn.unsqueeze(2).to_broadcast([128, 8, Lc])
            c0, s0 = tc[:, :, 0:Lc], tsn[:, :, 0, 0:Lc]
            c1, s1 = tc[:, :, Lc:2 * Lc], tsn[:, :, 0, Lc:2 * Lc]
            ta, tb = tmpA[:, :, 0:Lc], tmpB[:, :, 0:Lc]
            k4, k5 = [("fs", 4)], [("fs", 5)]
            self.tt("dve", ta, c0, pcb, ALU.mult, rk + tk, k4)
            self.tt("dve", tb, s0, psb, ALU.mult, rk + tk, k5)
            self.tt("dve", c1, ta, tb, ALU.subtract, k4 + k5, tk)
            self.tt("dve", ta, c0, psb, ALU.mult, rk + tk, k4)
            self.tt("dve", tb, s0, pcb, ALU.mult, rk + tk, k5)
            self.tt("dve", s1, ta, tb, ALU.add, k4 + k5, tk)
            V(q1, pc, pc, ALU.mult)
            V(q2, psn, psn, ALU.mult)
            V(q3, pc, psn, ALU.mult)
            V(pc, q1, q2, ALU.subtract)
            self.ts("dve", psn, q3, 2.0, None, ALU.mult, None, rk, rk)
            Lc *= 2
        self.ts("dve", tsn[:, :, 1, :], tsn[:, :, 0, :], -1.0, None, ALU.mult, None, tk, tk)
        for h in range(2):
            self.ts("dve", self.diagd[:, h, :], self.ident[:], self.vecs[:, h:h + 1], None, ALU.mult, None,
                    ["const", "vecs"], ["diagd"])

    def prefetch_x(self, ti):
        if ti >= len(self.tiles):
            return
        li, l, s, n = self.tiles[ti]
        row0 = s * SEQ + n * T
        src = self.x_in if li == 0 else self.xmid
        rd = [] if li == 0 else [("xmid", s, n)]
        self.dma("pool", self.xbn[:], src[row0:row0 + T, :].rearrange("(c p) d -> p c d", p=128), rd, ["xbn"])

    def seq_reset(self):
        self.memset("dve", self.carry[:], 0.0, [], ["carry"])
        self.memset("dve", self.r32[:], 0.0, [], ["r32"])
        self.memset("pool", self.rbf[:], 0.0, [], ["rbf"])

    def tile(self, l, s, n, first, last):
        ph = self.phases
        row0 = s * SEQ + n * T
        gA = ["gA", "gB"]
        src = self.x_in if first else self.xmid
        rd = [] if first else [("xmid", s, n)]
        self.dma("sp", self.xtok[:], src[row0:row0 + T, :].rearrange("(c p) d -> p c d", p=128), rd,
                 [("xtok", c) for c in range(4)])
        self.join("gB")
        for c in range(4):
            self.xT_chunk(c, self.xbn[:, c, :], ["xbn"])
        self.prefetch_x(self.ti + 1)
        self.phase_win(l, s, n)
        self.tap("zT", self.zT, [128, 12, T], BF16, gA + [("zT", j) for j in range(12)])
        self.tap("rkz", self.rkz, [128, 4, 256], BF16, gA + ["rkz"])
        self.tap("rvt", self.rvt, [128, 4, 256], BF16, gA + ["rvt"])
        self.tap("v1", self.v1[:, 0:4, :], [128, 4, 512], BF16, [("V1", c) for c in range(4)])
        if ph is None:
            g1 = self.s5_main_gen(l, s, n)
            g2 = self._chain(self.ret_gen(l, s, n), self.att_gen(l, s, n))
            alive1 = alive2 = True
            cyc = 0
            while alive1 or alive2:
                if alive1:
                    alive1 = next(g1, "end") != "end"
                for _ in range(3 if cyc % 2 == 0 else 2):
                    if alive2:
                        alive2 = next(g2, "end") != "end"
                cyc += 1
            self.s5_post(l, s, n)
        else:
            if "s5" in ph:
                self.phase_s5(l, s, n)
            if "ret" in ph:
                self.phase_ret(l, s, n)
            if "att" in ph:
                self.phase_att(l, s, n)
        self.tap("yT", self.yT[:], [128, 8, T], BF16, [("yT", j) for j in range(8)])
        if ph is None or "out" in ph:
            self.phase_outproj(l, s, n)
            self.tap("x1", self.xtok[:], [128, 4, D], F32, [("xtok", c) for c in range(4)])
        if ph is None or "mlp" in ph:
            self.join("gA")
            self.phase_mlp(l, s, n)
        dst = self.out if last else self.xmid
        wr = ["outdram"] if last else [("xmid", s, n)]
        self.dma("sp", dst[row0:row0 + T, :].rearrange("(c p) d -> p c d", p=128), self.xtok[:],
                 [("xtok", c) for c in range(4)], wr)

    @staticmethod
    def _chain(*gens):
        for g in gens:
            for _ in g:
                yield

    def xT_chunk(self, c, src_bf, src_keys):
        bk, bkey = self.bank()
        pb = bk[:].bitcast(BF16)
        for kt in range(8):
            self.tr(pb[:, kt * 128:(kt + 1) * 128], src_bf[:, kt * 128:(kt + 1) * 128], src_keys, [bkey])
        self.cp("act", self.xT[:, :, c * 128:(c + 1) * 128], pb[:, :].rearrange("p (k t) -> p k t", k=8),
                [bkey], [("xT", c)])

    def phase_win(self, l, s, n):
        gA = ["gA", "gB"]
        xTk = [("xT", c) for c in range(4)]
        win = self.w["w_in"][l]
        par = n % 2
        for b in range(6):
            ncols = 512 if b < 5 else 256
            buf, wkey = self.wblock(b)
            for jj in range(ncols // 128):
                j = 4 * b + jj
                wc = slice(jj * 128, (jj + 1) * 128)
                fm = None
                if j < 6:
                    fm = (self.zT[:, j, :], [("zT", j)], True)
                elif 8 <= j < 14:
                    fm = (self.zT[:, j - 2, :], [("zT", j - 2)], True)
                elif 14 <= j < 18:
                    fm = (self.khist[:, j - 14, n * T:(n + 1) * T], [("K", j - 14, n)], False)
                if fm is not None:
                    bk, bkey = self.bank()
                    for kt in range(8):
                        self.mm(bk[:, :], buf[:, kt, wc], self.xT[:, kt, :], kt == 0, kt == 7,
                                [wkey] + xTk, [bkey])
                    dest, wkeys, inA = fm
                    self.cp("act", dest, bk[:, :], [bkey] + (gA if inA else []), wkeys)
                tm = 4 <= j < 8 or j >= 18
                if tm:
                    bk, bkey = self.bank()
                    for c in range(4):
                        for kt in range(8):
                            self.mm(bk[:, c * 128:(c + 1) * 128], self.xT[:, kt, c * 128:(c + 1) * 128], buf[:, kt, wc],
                                    kt == 0, kt == 7, [wkey, ("xT", c)], [bkey])
                    if j < 6:
                        h0 = 2 * (j - 4)
                        for c in range(4):
                            self.tt("dve", self.rkz[:, c, (j - 4) * 128:(j - 3) * 128].rearrange("p (h d) -> p h d", h=2),
                                    bk[:, c * 128:(c + 1) * 128].rearrange("p (h d) -> p h d", h=2),
                                    self.cst["zeta"][:, h0:h0 + 2].unsqueeze(2).to_broadcast([128, 2, 64]),
                                    ALU.mult, [bkey, "const"] + gA, ["rkz"])
                    elif j < 8:
                        self.cp("act", self.rvt[:, :, (j - 6) * 128:(j - 5) * 128],
                                bk[:, :].rearrange("p (c d) -> p c d", c=4), [bkey] + gA, ["rvt"])
                    else:
                        self.cp("act", self.v1[:, 0:4, (j - 18) * 128:(j - 17) * 128],
                                bk[:, :].rearrange("p (c d) -> p c d", c=4), [bkey], [("V1", c) for c in range(4)])
        vs = self.vscr[par]
        v1k = [("V1", c) for c in range(4)]
        self.dma("sp", vs.rearrange("(c p) d -> p c d", p=128), self.v1[:, 0:4, :], v1k, [("vscr", par)])
        self.dma("sp", self.v4[:, par, :, :], vs.rearrange("(i r) d -> i r d", r=4), [("vscr", par)], [("V4", par)])
        self.dma("sp", self.v3[32 * n:32 * n + 32, :, :], vs.rearrange("(i r) d -> i r d", r=16), [("vscr", par)],
                 [("V3", n)])

    def s5_main_gen(self, l, s, n):
        gA = ["gA", "gB"]
        uT = [self.zT[:, 0, :], self.zT[:, 1, :]]
        ybk = [self.bank(hold=True), self.bank(hold=True)]
        self.s5_ybk = ybk
        mag = self.s5p[:, 5, :]
        v3d = lambda ap: ap.rearrange("p (a b) -> p a b", a=2)
        pending = []
        for c in range(4):
            cs = slice(c * 128, (c + 1) * 128)
            for h in range(2):
                yb, ykey = ybk[h]
                self.mm(yb[:, cs], self.diagd[:, h, :], uT[h][:, cs], True, False, ["diagd", ("zT", h)] + gA, [ykey])
            for gp0 in (0, 2, 4, 6):
                pair = (gp0, gp0 + 1)
                h = gp0 // 4
                pp = self.s5pp % 2
                self.s5pp += 1
                st = {}
                for gp in pair:
                    g = gp % 2
                    j = pp * 2 + g
                    bk, bkey = self.bank()
                    rd = ["btb", ("zT", h)] + gA
                    self.mm(bk[:, 0:128], self.btb[:, gp, 0, :], uT[h][:, cs], True, True, rd, [bkey])
                    self.mm(bk[:, 128:256], self.btb[:, gp, 1, :], uT[h][:, cs], True, True, rd, [bkey])
                    self.mm(bk[:, 256:384], self.btb[:, gp, 0, :], uT[h][:, cs], True, True, rd, [bkey])
                    st[gp] = dict(bk=bk, bkey=bkey, t1=self.s5t1[g], t2=self.s5t2[g], w=self.s5w[j], ta=self.s5ta[g],
                                  x=self.s5x[j], k1=("s5t1", g), k2=("s5t2", g), kw=("s5w", j), ka=("s5ta", g),
                                  kx=("s5x", j), cb=self.tabc[:, gp, :].unsqueeze(1).to_broadcast([128, 2, 128]),
                                  sbb=self.tabs[:, gp, :, :], magb=mag[:, gp:gp + 1].to_broadcast([128, 128]))
                while pending:
                    pending.pop(0)()
                for gp in pair:
                    d = st[gp]
                    self.tt("dve", v3d(d["t1"][:, :]), v3d(d["bk"][:, 0:256]), d["cb"], ALU.mult, [d["bkey"], "tab"] + gA, [d["k1"]])
                    self.tt("dve", v3d(d["t2"][:, :]), v3d(d["bk"][:, 128:384]), d["sbb"], ALU.mult, [d["bkey"], "tab"] + gA, [d["k2"]])
                for gp in pair:
                    d = st[gp]
                    self.tt("dve", d["t1"][:, :], d["t1"][:, :], d["t2"][:, :], ALU.add, [d["k1"], d["k2"]] + gA, [d["k1"]])
                for gp in pair:
                    d = st[gp]
                    w, v = d["w"], d["t1"]
                    self.scan(w[:, 0:128], d["magb"], v[:, 0:128], self.carry[:, 0, gp:gp + 1], [d["k1"], "carry", "s5prm"] + gA, [d["kw"]])
                    self.scan(w[:, 128:256], d["magb"], v[:, 128:256], self.carry[:, 1, gp:gp + 1], [d["k1"], "carry", "s5prm"] + gA, [d["kw"]])
                for gp in pair:
                    d = st[gp]
                    w = d["w"]
                    self.cp("act", w[:, 256:384], w[:, 0:128], [d["kw"]] + gA, [d["kw"]])
                    self.cp("act", self.wl[:, :, gp], w[:, 127:256:128], [d["kw"]] + gA, ["wl"])
                for gp in pair:
                    d = st[gp]
                    self.tt("pool", v3d(d["ta"][:, :]), v3d(d["w"][:, 0:256]), d["cb"], ALU.mult, [d["kw"], "tab"] + gA, [d["ka"]])
                for gp in pair:
                    d = st[gp]
                    self.tt("pool", v3d(d["w"][:, 128:384]), v3d(d["w"][:, 128:384]), d["sbb"], ALU.mult, [d["kw"], "tab"] + gA, [d["kw"]])
                for gp in pair:
                    d = st[gp]
                    self.tt("pool", d["x"][:, :], d["ta"][:, :], d["w"][:, 128:384], ALU.subtract, [d["ka"], d["kw"]] + gA, [d["kx"]])
                def cmm(pair=pair, st=st, h=h, cs=cs):
                    for gp in pair:
                        d = st[gp]
                        yb, ykey = ybk[h]
                        lastg = gp % 4 == 3
                        self.mm(yb[:, cs], self.ctb[:, gp, 0, :], d["x"][:, 0:128], False, False, ["ctb", d["kx"]] + gA, [ykey])
                        self.mm(yb[:, cs], self.ctb[:, gp, 1, :], d["x"][:, 128:256], False, lastg, ["ctb", d["kx"]] + gA, [ykey])
                pending.append(cmm)
                yield
            while pending:
                pending.pop(0)()
            cl = self.tabc[:, :, 127]
            sl = self.tabs[:, :, 0, 127]
            wr_, wi_ = self.wl[:, 0, :], self.wl[:, 1, :]
            a_, b_ = self.s5p[:, 16, :], self.s5p[:, 17, :]
            c_, d_ = self.s5p[:, 18, :], self.s5p[:, 19, :]
            self.tt("dve", a_, cl, wr_, ALU.mult, ["tab", "wl"], ["s5tmpa"])
            self.tt("dve", b_, sl, wi_, ALU.mult, ["tab", "wl"], ["s5tmpb"])
            self.tt("dve", c_, cl, wi_, ALU.mult, ["tab", "wl"], ["s5tmpc"])
            self.tt("dve", d_, sl, wr_, ALU.mult, ["tab", "wl"], ["s5tmpd"])
            self.tt("dve", self.carry[:, 0, :], a_, b_, ALU.subtract, ["s5tmpa", "s5tmpb"], ["carry"])
            self.tt("dve", self.carry[:, 1, :], c_, d_, ALU.add, ["s5tmpc", "s5tmpd"], ["carry"])
            yield

    def phase_s5(self, l, s, n):
        for _ in self.s5_main_gen(l, s, n):
            pass
        self.s5_post(l, s, n)

    def s5_post(self, l, s, n):
        ybk = self.s5_ybk
        g32 = [self.fs[0], self.fs[1]]
        gbf = [self.bs[0], self.bs[1]]
        for h in range(2):
            yb, ykey = ybk[h]
            self.act(g32[h][:, :], yb[:, :], AF.Gelu_apprx_tanh, [ykey], [("fs", h)])
            self.act(gbf[h][:, :], yb[:, :], AF.Gelu_apprx_tanh, [ykey], [("bs", h)])
        self.release(ybk[0][1], ybk[1][1])
        self.tap("s5g", self.fs[0][:, :], [128, T], F32, [("fs", 0)])
        o32 = [self.fs[2], self.fs[3]]
        sqb = [self.bs[2], self.bs[3]]
        for m in range(2):
            bk, bkey = self.bank()
            for kt in range(2):
                self.mm(bk[:, :], self.wglu[:, kt, m * 128:(m + 1) * 128], gbf[kt][:, :], kt == 0, kt == 1,
                        ["wglu", ("bs", kt)], [bkey])
            self.act(o32[m][:, :], bk[:, :], AF.Sigmoid, [bkey, "vecs"], [("fs", 2 + m)], bias=self.vecs[:, 2 + m:3 + m])
            self.tt("dve", o32[m][:, :], o32[m][:, :], g32[m][:, :], ALU.mult, [("fs", 2 + m), ("fs", m)], [("fs", 2 + m)])
            self.act(sqb[m][:, :], o32[m][:, :], AF.Square, [("fs", 2 + m)], [("bs", 2 + m)])
        bk, bkey = self.bank()
        for m in range(2):
            self.mm(bk[:, :], self.cst["ones"][:], sqb[m][:, :], m == 0, m == 1, ["const", ("bs", 2 + m)], [bkey])
        rs = self.fs[4]
        self.rsqrt_act(rs[:, :], bk[:, :], 1.0 / 256, [bkey], [("fs", 4)])
        for m in range(2):
            self.stt(self.yT[:, m, :], o32[m][:, :], self.vecs[:, 4 + m:5 + m], rs[:, :], ALU.mult, ALU.mult,
                     [("fs", 2 + m), ("fs", 4), "vecs"], [("yT", m)])

    def phase_ret(self, l, s, n):
        for _ in self.ret_gen(l, s, n):
            pass

    def ret_gen(self, l, s, n):
        gA = ["gA", "gB"]
        rqT = [self.zT[:, 2, :], self.zT[:, 3, :]]
        rkT = [self.zT[:, 4, :], self.zT[:, 5, :]]
        rgT = [self.zT[:, 6, :], self.zT[:, 7, :]]
        C = self.cst
        for hp in range(2):
            self.tt("dve", self.rqx[:, hp, :].rearrange("p (c q) -> p c q", c=4),
                    rqT[hp].rearrange("p (c q) -> p c q", c=4),
                    C["xi"][:, hp, :].unsqueeze(1).to_broadcast([128, 4, 128]), ALU.mult,
                    [("zT", 2 + hp), "const"] + gA, [("rqx", hp)])
        obk = [self.bank(hold=True), self.bank(hold=True)]

        def st_a(c):
            cs = slice(c * 128, (c + 1) * 128)
            sb2 = [self.bank(), self.bank()]
            for h in range(4):
                hp, h2 = h // 2, h % 2
                pr = slice(64 * h2, 64 * h2 + 64)
                sbk, skey = sb2[h2]
                self.mm(sbk[:, hp * 128:(hp + 1) * 128], rkT[hp][pr, cs], rqT[hp][pr, cs], True, True,
                        [("zT", 4 + hp), ("zT", 2 + hp)] + gA, [skey])
            pt = self.bs[4 + (c % 2)]
            pk = ("bs", 4 + (c % 2))
            for h2 in range(2):
                sbk, skey = sb2[h2]
                self.tt("dve", pt[:, h2 * 256:(h2 + 1) * 256], sbk[:, 0:256],
                        C["dr"][:, h2].rearrange("p a q -> p (a q)"), ALU.mult, [skey, "const", pk], [pk])

        def st_b(c):
            cs = slice(c * 128, (c + 1) * 128)
            pt = self.bs[4 + (c % 2)]
            pk = ("bs", 4 + (c % 2))
            for h in range(4):
                hp, h2 = h // 2, h % 2
                pr = slice(64 * h2, 64 * h2 + 64)
                ob, okey = obk[hp]
                pcol = slice(h2 * 256 + hp * 128, h2 * 256 + hp * 128 + 128)
                self.mm(ob[pr, cs], self.rvt[:, c, h * 64:(h + 1) * 64], pt[:, pcol], True, False,
                        ["rvt", pk] + gA, [okey])
                self.mm(ob[pr, cs], self.rbf[pr, hp, :], self.rqx[pr, hp, cs], False, True,
                        ["rbf", ("rqx", hp)] + gA, [okey], tile_position=(64 * h2, 64 * h2))

        def st_u(c):
            kvb, kvkey = self.bank()
            for h in range(4):
                hp, h2 = h // 2, h % 2
                pr = slice(64 * h2, 64 * h2 + 64)
                self.mm(kvb[pr, hp * 64:(hp + 1) * 64], self.rkz[:, c, h * 64:(h + 1) * 64], self.rvt[:, c, h * 64:(h + 1) * 64],
                        True, True, ["rkz", "rvt"] + gA, [kvkey])
            for hp in range(2):
                self.stt(self.r32[:, hp, :], self.r32[:, hp, :], C["gch"][:, hp:hp + 1], kvb[:, hp * 64:(hp + 1) * 64],
                         ALU.mult, ALU.add, ["r32", kvkey, "const"], ["r32"])
            self.cp("act", self.rbf[:], self.r32[:], ["r32"], ["rbf"])

        st_a(0)
        for c in range(4):
            if c + 1 < 4:
                st_a(c + 1)
                yield
            st_b(c)
            st_u(c)
            yield
        for hp in range(2):
            ob, okey = obk[hp]
            o32, obf, osq = self.fs[0], self.bs[0], self.bs[1]
            self.cp("act", o32[:, :], ob[:, :], [okey], [("fs", 0)])
            self.cp("act", obf[:, :], ob[:, :], [okey], [("bs", 0)])
            self.act(osq[:, :], ob[:, :], AF.Square, [okey], [("bs", 1)])
            mb, mkey = self.bank()
            self.mm(mb[:, :], C["blk64"][:], obf[:, :], True, True, ["const", ("bs", 0)], [mkey])
            eb, ekey = self.bank()
            self.mm(eb[:, :], C["blk64"][:], osq[:, :], True, True, ["const", ("bs", 1)], [ekey])
            mean, var = self.fs[1], self.fs[2]
            self.cp("act", mean[:, :], mb[:, :], [mkey], [("fs", 1)])
            self.tt("dve", var[:, :], mean[:, :], mean[:, :], ALU.mult, [("fs", 1)], [("fs", 2)])
            self.tt("dve", var[:, :], eb[:, :], var[:, :], ALU.subtract, [ekey, ("fs", 2)], [("fs", 2)])
            self.ts("dve", var[:, :], var[:, :], 0.0, None, ALU.max, None, [("fs", 2)], [("fs", 2)])
            self.rsqrt_act(var[:, :], var[:, :], 1.0, [("fs", 2)], [("fs", 2)])
            self.tt("dve", o32[:, :], o32[:, :], mean[:, :], ALU.subtract, [("fs", 0), ("fs", 1)], [("fs", 0)])
            self.tt("dve", o32[:, :], o32[:, :], var[:, :], ALU.mult, [("fs", 0), ("fs", 2)], [("fs", 0)])
            sg = self.fs[3]
            self.act(sg[:, :], rgT[hp], AF.Silu, [("zT", 6 + hp)] + gA, [("fs", 3)])
            self.stt(self.yT[:, 2 + hp, :], o32[:, :], self.vecs[:, 6 + hp:7 + hp], sg[:, :], ALU.mult, ALU.mult,
                     [("fs", 0), ("fs", 3), "vecs"], [("yT", 2 + hp)])
            yield
        self.release(obk[0][1], obk[1][1])

    def phase_att(self, l, s, n):
        for _ in self.att_gen(l, s, n):
            pass

    def att_gen(self, l, s, n):
        gA = ["gA", "gB"]
        C = self.cst
        ones64 = C["ones"][:, 0:64]
        self.nE = getattr(self, "nE", 0)
        o32 = [self.fs[i] for i in range(4)]
        for hp in range(4):
            aq = self.zT[:, 8 + hp, :]
            aqk = [("zT", 8 + hp)] + gA
            ob, okey = self.bank(hold=True)
            lb, lkey = self.bank(hold=True)
            started = [False, False]

            def pv(vrows, pt, pk, item_cols, out_cols, ksz, h2, vkeys):
                pr = slice(64 * h2, 64 * h2 + 64)
                st = not started[h2]
                started[h2] = True
                self.mm(ob[pr, out_cols], vrows, pt[0:ksz, item_cols], st, False,
                        list(vkeys) + [pk], [okey], tile_position=(0, 64 * h2))
                self.mm(lb[pr, out_cols], ones64[0:ksz, :], pt[0:ksz, item_cols], st, False,
                        ["const", pk], [lkey], tile_position=(0, 64 * h2))

            def hcol(h2):
                return slice((2 * hp + h2) * 64, (2 * hp + h2) * 64 + 64)

            def scratch():
                i = self.nE % 2
                self.nE += 1
                E = [(self.bs[4 * i + 0], ("bs", 4 * i + 0)), (self.bs[4 * i + 1], ("bs", 4 * i + 1))]
                Pb = [(self.bs[4 * i + 2], ("bs", 4 * i + 2)), (self.bs[4 * i + 3], ("bs", 4 * i + 3))]
                return E, Pb

            units = []

            def make_unit12(br, pair):
                dtab = C["d1"] if br == 1 else C["d2"]
                us = [2 * pair, 2 * pair + 1]
                valid = []
                for ui, u in enumerate(us):
                    for rel in (0, 1):
                        if br == 1 and 4 * n + u - rel < 0:
                            continue
                        if br == 2 and n - rel < 0:
                            continue
                        valid.append((ui, u, rel))
                stt_ = {}

                def stage_a():
                    sb2 = [self.bank(), self.bank()]
                    E, Pb = scratch()
                    stt_["Pb"] = Pb
                    for h2 in range(2):
                        pr = slice(64 * h2, 64 * h2 + 64)
                        sbk, skey = sb2[h2]
                        for ui, u, rel in valid:
                            it = ui * 2 + rel
                            if br == 1:
                                gk = 4 * n + u - rel
                                kap = self.khist[pr, hp, gk * 128:(gk + 1) * 128]
                                qap = aq[pr, u * 128:(u + 1) * 128]
                                kkey = ("K", hp, gk // 4)
                            else:
                                tn = n - rel
                                kap = self.khist[pr, hp, tn * T + u:(tn + 1) * T:4]
                                qap = aq[pr, u:T:4]
                                kkey = ("K", hp, tn)
                            self.mm(sbk[:, it * 128:(it + 1) * 128], kap, qap, True, True, [kkey] + aqk, [skey])
                        eb, ek = E[h2]
                        pt, pk = Pb[h2]
                        if len(valid) == 4:
                            self.act(eb[:, :], sbk[:, :], AF.Exp, [skey], [ek], scale=0.125)
                        else:
                            self.memset("pool", eb[:, :], 0.0, [ek], [ek])
                            for ui, u, rel in valid:
                                it = ui * 2 + rel
                                self.act(eb[:, it * 128:(it + 1) * 128], sbk[:, it * 128:(it + 1) * 128], AF.Exp,
                                         [skey, ek], [ek], scale=0.125)
                        self.tt("dve", pt[:, :].rearrange("p (u x) -> p u x", u=2),
                                eb[:, :].rearrange("p (u x) -> p u x", u=2),
                                dtab[:, hp, h2].rearrange("p a q -> p (a q)").unsqueeze(1).to_broadcast([128, 2, 256]),
                                ALU.mult, [ek, "const"], [pk])

                def stage_b():
                    Pb = stt_["Pb"]
                    for h2 in range(2):
                        pt, pk = Pb[h2]
                        for ui, u, rel in valid:
                            it = ui * 2 + rel
                            if br == 1:
                                slot = u - rel if u - rel >= 0 else 4
                                pv(self.v1[:, slot, hcol(h2)], pt, pk, slice(it * 128, (it + 1) * 128),
                                   slice(u * 128, (u + 1) * 128), 128, h2, [("V1", slot)])
                            else:
                                vpar = (n - rel) % 2
                                pv(self.v4[:, vpar, u, hcol(h2)], pt, pk, slice(it * 128, (it + 1) * 128),
                                   slice(u, T, 4), 128, h2, [("V4", vpar)])
                return stage_a, stage_b

            def make_unit3():
                nk = 32 * (n + 1)
                stt_ = {}

                def stage_a():
                    sb2 = [self.bank(), self.bank()]
                    E, Pb = scratch()
                    stt_["Pb"] = Pb
                    kk3 = [("K", hp, t_) for t_ in range(n + 1)]
                    for h2 in range(2):
                        pr = slice(64 * h2, 64 * h2 + 64)
                        sbk, skey = sb2[h2]
                        for r in range(16):
                            self.mm(sbk[0:nk, r * 32:(r + 1) * 32], self.khist[pr, hp, r:(n + 1) * T:16],
                                    aq[pr, r:T:16], True, True, kk3 + aqk, [skey])
                        eb, ek = E[h2]
                        pt, pk = Pb[h2]
                        self.act(eb[0:nk, :], sbk[0:nk, :], AF.Exp, [skey], [ek], scale=0.125)
                        self.tt("dve", pt[0:nk, :].rearrange("p (r q) -> p r q", r=16),
                                eb[0:nk, :].rearrange("p (r q) -> p r q", r=16),
                                C["d3"][0:nk, hp, h2, 32 * n:32 * n + 32].unsqueeze(1).to_broadcast([nk, 16, 32]),
                                ALU.mult, [ek, "const"], [pk])

                def stage_b():
                    Pb = stt_["Pb"]
                    v3k = [("V3", t_) for t_ in range(n + 1)]
                    for h2 in range(2):
                        pt, pk = Pb[h2]
                        for r in range(16):
                            pv(self.v3[0:nk, r, hcol(h2)], pt, pk, slice(r * 32, (r + 1) * 32), slice(r, T, 16), nk, h2, v3k)
                return stage_a, stage_b

            for br in (1, 2):
                for pair in range(2):
                    units.append(make_unit12(br, pair))
            units.append(make_unit3())
            units[0][0]()
            for k in range(len(units)):
                if k + 1 < len(units):
                    units[k + 1][0]()
                    yield
                units[k][1]()
                yield
            rl = self.fs[4]
            self.recip_act(rl[:, :], lb[:, :], [lkey], [("fs", 4)])
            self.tt("dve", o32[hp][:, :], ob[:, :], rl[:, :], ALU.mult, [okey, ("fs", 4)], [("fs", hp)])
            self.release(okey, lkey)
            yield
        self.cp("pool", self.v1[:, 4, :], self.v1[:, 3, :], [("V1", 3)], [("V1", 4)])
        bk, bkey = self.bank()
        for hp in range(4):
            sq, sk = self.bs[hp % 2], ("bs", hp % 2)
            self.act(sq[:, :], o32[hp][:, :], AF.Square, [("fs", hp)], [sk])
            self.mm(bk[:, :], C["ones"][:], sq[:, :], hp == 0, hp == 3, ["const", sk], [bkey])
        rs = self.fs[4]
        self.rsqrt_act(rs[:, :], bk[:, :], 1.0 / 512, [bkey], [("fs", 4)])
        for hp in range(4):
            self.stt(self.yT[:, 4 + hp, :], o32[hp][:, :], self.vecs[:, 8 + hp:9 + hp], rs[:, :], ALU.mult, ALU.mult,
                     [("fs", hp), ("fs", 4), "vecs"], [("yT", 4 + hp)])

    def layer_norm(self, c, which):
        xk = ("xtok", c)
        xc = self.xtok[:, c, :]
        i = self.lnrot % 2
        self.lnrot += 1
        st = self.bnst[:, i]
        sk, lk = ("bnst", i), ("lns", i)
        for j in range(2):
            self.p.add("dve", (lambda j=j: (lambda e: e.bn_stats(out=st[:, j, :], in_=xc[:, j * 512:(j + 1) * 512])))(),
                       [xk], [sk])
        lns = self.lns[:, i, :]
        mv = lns[:, 0:2]
        self.p.add("dve", lambda e: e.bn_aggr(out=mv, in_=st.rearrange("p a b -> p (a b)")), [sk], [lk])
        rstd = lns[:, 2:3]
        self.rsqrt_act(rstd, lns[:, 1:2], 1.0, [lk], [lk])
        self.stt(xc, xc, lns[:, 0:1], self.lnp[:, 0, :], ALU.subtract, ALU.mult, [xk, lk, ("lnp", which)], [xk])
        self.stt(xc, xc, rstd, self.lnp[:, 1, :], ALU.mult, ALU.add, [xk, lk, ("lnp", which)], [xk])

    def load_lnp(self, l, which):
        self.dma("sp", self.lnp[:, :, :], self.prm["lnp"][l, 2 * which:2 * which + 2].rearrange("a p d -> p a d"),
                 [], [("lnp", 0), ("lnp", 1)])

    def phase_outproj(self, l, s, n):
        self.load_lnp(l, 0)
        bufs = [self.wblock(6, look=2), self.wblock(7, look=1)]
        yk = [("yT", j) for j in range(8)]

        def x1T(c):
            self.cp("act", self.xb[:], self.xtok[:, c, :], [("xtok", c)], ["xb"])
            self.xT_chunk(c, self.xb, ["xb"])

        for c in range(4):
            cs = slice(c * 128, (c + 1) * 128)
            for h in range(2):
                buf, wkey = bufs[h]
                bk, bkey = self.bank()
                for kt in range(8):
                    self.mm(bk[:, :], self.yT[:, kt, cs], buf[:, kt, :], kt == 0, kt == 7, [wkey] + yk, [bkey])
                xs = self.xtok[:, c, h * 512:(h + 1) * 512]
                self.stt(xs, xs, ALPHA, bk[:, :], ALU.mult, ALU.add, [("xtok", c), bkey], [("xtok", c)])
            self.layer_norm(c, 0)
            if c >= 1:
                x1T(c - 1)
        x1T(3)

    def phase_mlp(self, l, s, n):
        gA = ["gA", "gB"]
        w1 = self.w["mlp_w1"][l]
        w2 = self.w["mlp_w2"][l]
        self.load_lnp(l, 1)
        xTk = [("xT", c) for c in range(4)]
        for b in range(8):
            buf, wkey = self.wblock(8 + b)
            for m in range(4):
                bk, bkey = self.bank()
                for kt in range(8):
                    self.mm(bk[:, :], buf[:, kt, m * 128:(m + 1) * 128], self.xT[:, kt, :], kt == 0, kt == 7,
                            [wkey] + xTk, [bkey])
                tmp, tk = self.bs[(4 * b + m) % 4], ("bs", (4 * b + m) % 4)
                self.act(tmp[:, :], bk[:, :], AF.Relu, [bkey], [tk])
                self.tt("dve", self.hidden[:, 4 * b + m, :], tmp[:, :], tmp[:, :], ALU.mult, [tk] + gA, [("hid", 4 * b + m)])
        for h in range(2):
            banks = [self.bank(hold=True) for _ in range(4)]
            for q in range(4):
                buf, wkey = self.wblock(16 + 4 * h + q)
                for c in range(4):
                    bk, bkey = banks[c]
                    for kt in range(8):
                        kk = q * 8 + kt
                        self.mm(bk[:, :], self.hidden[:, kk, c * 128:(c + 1) * 128], buf[:, kt, :], kk == 0, kk == 31,
                                [wkey, ("hid", kk)] + gA, [bkey])
            for c in range(4):
                bk, bkey = banks[c]
                xs = self.xtok[:, c, h * 512:(h + 1) * 512]
                self.stt(xs, xs, ALPHA, bk[:, :], ALU.mult, ALU.add, [("xtok", c), bkey], [("xtok", c)])
            self.release(*[k for _, k in banks])
        for c in range(4):
            self.layer_norm(c, 1)


_CACHE = {}


def _get_program(layers, nseq=NSEQ, ntiles=NT, taps=(), phases=None):
    key = (tuple(layers), nseq, ntiles, tuple(sorted(taps)), None if phases is None else tuple(sorted(phases)))
    if key not in _CACHE:
        b = Builder(layers, nseq, ntiles, taps, phases)
        b.epsb = b.sb("epsb", [128, 1], F32)
        b.memset("dve", b.epsb[:], EPS, [], ["epsb_k"])
        nc = b.build()
        _CACHE[key] = (nc, b)
    return _CACHE[key]


def make_in_maps(inputs, nseq=NSEQ, ncores=8):
    x = np.ascontiguousarray(inputs["x"], dtype=np.float32)
    consts = make_consts()
    prm = layout_params(inputs)
    shared = {}
    for name, _ in WEIGHT_SPECS:
        shared[name] = np.ascontiguousarray(inputs[name], dtype=np.float32)
    for k, v in prm.items():
        shared[k] = v
    for k, v in consts.items():
        shared["c_" + k] = v
    maps = []
    for c in range(ncores):
        m = dict(shared)
        m["x"] = x[c * nseq:(c + 1) * nseq].reshape(nseq * SEQ, D)
        maps.append(m)
    return maps


LAUNCH_PLAN = [[0, 1]]


def kernel(**inputs):
    inputs = {k: np.asarray(v) for k, v in inputs.items()}
    x = inputs["x"]
    cur = dict(inputs)
    for layers in LAUNCH_PLAN:
        nc, b = _get_program(layers)
        maps = make_in_maps(cur)
        res = run_bass_kernel_spmd(nc, maps, core_ids=list(range(8)))
        out = np.stack([r["out"].reshape(NSEQ, SEQ, D) for r in res.results], axis=0).reshape(16, SEQ, D)
        cur = dict(inputs)
        cur["x"] = out
    return out.astype(np.float32)
```

```python
import math
from contextlib import ExitStack
import numpy as np
import ml_dtypes
import concourse.bass as bass
import concourse.mybir as mybir
from concourse.bass_utils import run_bass_kernel_spmd

F32 = mybir.dt.float32
BF16 = mybir.dt.bfloat16
AF = mybir.ActivationFunctionType
ALU = mybir.AluOpType

D = 1024
SEQ = 2048
NSEQ = 2
T = 512
NT = SEQ // T
INW = 2816
DFF = 4096
DEPTH = 2
ALPHA = (2 * DEPTH) ** 0.25
EPS = 1e-5
NW = 3
OUTROT_ENG = "dve"


class Op:
    __slots__ = ("eng", "fn", "deps", "dma", "need_inc", "cnt", "dsem", "dval", "idx")


class Prog:
    COMPUTE = ("pe", "act", "dve", "pool")

    def __init__(self):
        self.ops = []
        self.res = {}

    def add(self, eng, fn, reads=(), writes=(), dma=False):
        idx = len(self.ops)
        deps = set()
        for k in reads:
            st = self.res.get(k)
            if st is None:
                st = self.res[k] = [None, {}, []]
            if st[0] is not None:
                deps.add(st[0])
        for k in writes:
            st = self.res.get(k)
            if st is None:
                st = self.res[k] = [None, {}, []]
            if st[0] is not None:
                deps.add(st[0])
            deps.update(st[1].values())
            deps.update(st[2])
        for k in reads:
            st = self.res[k]
            if dma:
                st[2].append(idx)
            else:
                st[1][eng] = idx
        for k in writes:
            self.res[k] = [idx, {}, []]
        deps.discard(idx)
        o = Op()
        o.eng, o.fn, o.deps, o.dma, o.idx = eng, fn, deps, dma, idx
        o.need_inc = False
        o.cnt = 0
        o.dsem = None
        o.dval = 0
        self.ops.append(o)
        return idx

    def emit(self, nc, stack, n_dma_sems=None):
        n_dma_sems = n_dma_sems or {"sp": 14, "pool": 8, "act": 4}
        ops = self.ops
        for o in ops:
            for p in o.deps:
                P = ops[p]
                if P.dma:
                    continue
                if P.eng == "pe" and o.eng == "pe" and not o.dma:
                    continue
                P.need_inc = True
        engsem = {e: stack.enter_context(nc.semaphore("s_" + e)) for e in self.COMPUTE}
        cnt = {e: 0 for e in self.COMPUTE}
        dsems = {}
        dstate = {}
        for q, n in n_dma_sems.items():
            dsems[q] = [stack.enter_context(nc.semaphore("d_%s%d" % (q, i))) for i in range(n)]
            dstate[q] = [0, [0] * n]
        for o in ops:
            if o.dma:
                st = dstate[o.eng]
                i = st[0] % len(dsems[o.eng])
                st[0] += 1
                st[1][i] += 16
                o.dsem = dsems[o.eng][i]
                o.dval = st[1][i]
            elif o.need_inc:
                cnt[o.eng] += 1
                o.cnt = cnt[o.eng]
        byeng = {e: [] for e in ("pe", "act", "dve", "pool", "sp")}
        for o in ops:
            byeng[o.eng].append(o)
        final_waits = []
        for q in dsems:
            for i, s in enumerate(dsems[q]):
                if dstate[q][1][i] > 0:
                    final_waits.append((s, dstate[q][1][i]))

        def run(engname, e):
            waited = {}
            for o in byeng[engname]:
                waits = {}
                for p in o.deps:
                    P = ops[p]
                    if P.dma:
                        sem, val = P.dsem, P.dval
                    else:
                        if P.eng == "pe" and engname == "pe" and not o.dma:
                            continue
                        sem, val = engsem[P.eng], P.cnt
                    key = id(sem)
                    if key not in waits or waits[key][1] < val:
                        waits[key] = (sem, val)
                if o.dma and o.dval > 16:
                    key = id(o.dsem)
                    v = o.dval - 16
                    if key not in waits or waits[key][1] < v:
                        waits[key] = (o.dsem, v)
                for key, (sem, val) in waits.items():
                    if waited.get(key, 0) < val:
                        e.wait_ge(sem, val)
                        waited[key] = val
                ins = o.fn(e)
                if o.dma:
                    ins.then_inc(o.dsem, 16)
                elif o.need_inc:
                    ins.then_inc(engsem[engname], 1)
            if engname == "sp":
                for sem, val in final_waits:
                    e.wait_ge(sem, val)

        with nc.Block() as block:
            @block.tensor
            def _(e):
                run("pe", e)

            @block.scalar
            def _(e):
                run("act", e)

            @block.vector
            def _(e):
                run("dve", e)

            @block.gpsimd
            def _(e):
                run("pool", e)

            @block.sync
            def _(e):
                run("sp", e)


def make_consts():
    bf = ml_dtypes.bfloat16
    c = {}
    c["ident"] = np.eye(128, dtype=np.float32).astype(bf)
    c["ones"] = np.ones((128, 128), np.float32).astype(bf)
    blk = np.zeros((128, 128), np.float32)
    blk[:64, :64] = 1.0 / 64
    blk[64:, 64:] = 1.0 / 64
    c["blk64"] = blk.astype(bf)
    slopes = 2.0 ** (-8.0 * np.arange(1, 9, dtype=np.float64) / 8)
    k = np.arange(128, dtype=np.float64)[:, None]
    q = np.arange(128, dtype=np.float64)[None, :]

    def dtab(dil):
        t = np.zeros((128, 8, 2, 128), np.float64)
        for h in range(8):
            dc = q - k
            t[:, h, 0, :] = np.where(dc >= 0, np.exp(-slopes[h] * dil * np.maximum(dc, 0)), 0.0)
            dp = 128 + q - k
            t[:, h, 1, :] = np.where(dp <= 128, np.exp(-slopes[h] * dil * dp), 0.0)
        return t.reshape(128, 4, 2, 2, 128).astype(np.float32).astype(bf)

    c["d1"] = dtab(1)
    c["d2"] = dtab(4)
    d3 = np.zeros((128, 8, 128), np.float64)
    for h in range(8):
        dc = q - k
        d3[:, h, :] = np.where(dc >= 0, np.exp(-slopes[h] * 16 * np.maximum(dc, 0)), 0.0)
    c["d3"] = d3.reshape(128, 4, 2, 128).astype(np.float32).astype(bf)
    gam = 1.0 - 2.0 ** (-5.0 - np.arange(4, dtype=np.float64))
    dr = np.zeros((128, 2, 2, 128), np.float64)
    for h in range(4):
        dc = q - k
        dr[:, h % 2, h // 2, :] = np.where(dc >= 0, gam[h] ** np.maximum(dc, 0), 0.0) / 8.0
    c["dr"] = dr.astype(np.float32).astype(bf)
    zeta = np.zeros((128, 4), np.float64)
    for h in range(4):
        zeta[:, h] = gam[h] ** (127 - np.arange(128)) / 8.0
    c["zeta"] = zeta.astype(np.float32)
    xi = np.zeros((128, 2, 128), np.float64)
    gch = np.zeros((128, 2), np.float64)
    for h in range(4):
        hp, h2 = h // 2, h % 2
        xi[64 * h2:64 * h2 + 64, hp, :] = (gam[h] ** (np.arange(128) + 1.0))[None, :]
        gch[64 * h2:64 * h2 + 64, hp] = gam[h] ** 128
    c["xi"] = xi.astype(np.float32).astype(bf)
    c["gch"] = gch.astype(np.float32)
    return c


CONST_SPECS = [
    ("ident", [128, 128], BF16), ("ones", [128, 128], BF16), ("blk64", [128, 128], BF16),
    ("d1", [128, 4, 2, 2, 128], BF16), ("d2", [128, 4, 2, 2, 128], BF16),
    ("d3", [128, 4, 2, 128], BF16), ("dr", [128, 2, 2, 128], BF16),
    ("zeta", [128, 4], F32), ("xi", [128, 2, 128], BF16), ("gch", [128, 2], F32),
]

PARAM_SPECS = [
    ("bt", [DEPTH, 128, 8, 2, 128], F32),
    ("ct", [DEPTH, 128, 8, 2, 128], F32),
    ("lam", [DEPTH, 128, 3, 8], F32),
    ("vecs", [DEPTH, 128, 12], F32),
    ("lnp", [DEPTH, 4, 128, 1024], F32),
]


def layout_params(inp):
    L = DEPTH
    bt = np.zeros((L, 128, 8, 2, 128), np.float32)
    ct = np.zeros((L, 128, 8, 2, 128), np.float32)
    for g in range(16):
        gp, g2, g8 = g // 2, g % 2, g % 8
        for ri, (bname, cname) in enumerate((("ssm_b_re", "ssm_c_re"), ("ssm_b_im", "ssm_c_im"))):
            b = inp[bname][:, g]
            bt[:, g8 * 16:(g8 + 1) * 16, gp, ri, g2 * 64:(g2 + 1) * 64] = np.transpose(b, (0, 2, 1))
            cc = inp[cname][:, g]
            ct[:, g2 * 64:(g2 + 1) * 64, gp, ri, g8 * 16:(g8 + 1) * 16] = np.transpose(cc, (0, 2, 1))
    lam = np.zeros((L, 128, 3, 8), np.float32)
    for g in range(16):
        gp, g2 = g // 2, g % 2
        lam[:, g2 * 64:(g2 + 1) * 64, 0, gp] = inp["ssm_lambda_re"][:, g]
        lam[:, g2 * 64:(g2 + 1) * 64, 1, gp] = inp["ssm_lambda_im"][:, g]
        lam[:, g2 * 64:(g2 + 1) * 64, 2, gp] = inp["ssm_log_dt"][:, g][:, None]
    vecs = np.zeros((L, 128, 12), np.float32)
    vecs[:, :, 0:2] = inp["ssm_d"].reshape(L, 2, 128).transpose(0, 2, 1)
    vecs[:, :, 2:4] = inp["ssm_b_glu"].reshape(L, 2, 128).transpose(0, 2, 1)
    vecs[:, :, 4:6] = inp["ssm_out_norm"].reshape(L, 2, 128).transpose(0, 2, 1)
    vecs[:, :, 6:8] = inp["ret_out_norm"].reshape(L, 2, 128).transpose(0, 2, 1)
    vecs[:, :, 8:12] = inp["attn_out_norm"].reshape(L, 4, 128).transpose(0, 2, 1)
    lnp = np.stack([inp["ln1_w"], inp["ln1_b"], inp["ln2_w"], inp["ln2_b"]], axis=1)
    lnp = np.ascontiguousarray(np.broadcast_to(lnp[:, :, None, :], (L, 4, 128, 1024))).astype(np.float32)
    return {"bt": bt, "ct": ct, "lam": lam, "vecs": vecs, "lnp": lnp}


WEIGHT_SPECS = [
    ("w_in", [DEPTH, D, INW]), ("w_out", [DEPTH, D, D]), ("mlp_w1", [DEPTH, D, DFF]),
    ("mlp_w2", [DEPTH, DFF, D]), ("ssm_w_glu", [DEPTH, 256, 256]),
]


class Builder:
    def __init__(self, layers, nseq=NSEQ, ntiles=NT, taps=(), phases=None):
        self.layers = list(layers)
        self.nseq = nseq
        self.ntiles = ntiles
        self.taps = set(taps)
        self.phases = phases
        self.nc = bass.Bass("TRN2", target_bir_lowering=False)
        self.p = Prog()
        self.stack = ExitStack()
        self.tap_specs = {}
        self.psrot = 0
        self.wrot = 0
        self.dummy_n = 0
        self.held = set()
        self.lnrot = 0
        self.s5pp = 0

    def sb(self, name, shape, dt):
        return self.stack.enter_context(self.nc.sbuf_tensor("sb_" + name, list(shape), dt))

    def dram_in(self, name, shape, dt):
        return self.nc.dram_tensor(name, list(shape), dt, kind="ExternalInput").ap()

    def dram_out(self, name, shape, dt):
        return self.nc.dram_tensor(name, list(shape), dt, kind="ExternalOutput").ap()

    def mm(self, out, lhsT, rhs, start, stop, reads, writes, **kw):
        rb, cb = lhsT.base_partition(), out.base_partition()
        if (rb or cb) and "tile_position" not in kw:
            kw["tile_position"] = (rb, cb)
        self.p.add("pe", lambda e: e.matmul(out, lhsT=lhsT, rhs=rhs, start=start, stop=stop, **kw), reads, writes)

    def tr(self, out, in_, reads, writes):
        ident = self.ident[:]
        self.p.add("pe", lambda e: e.transpose(out=out, in_=in_, identity=ident), list(reads) + ["const"], writes)

    def act(self, out, in_, func, reads, writes, **kw):
        self.p.add("act", lambda e: e.activation(out=out, in_=in_, func=func, **kw), list(reads) + ["epsb_k"], writes)

    def tt(self, eng, out, in0, in1, op, reads, writes):
        self.p.add(eng, lambda e: e.tensor_tensor(out=out, in0=in0, in1=in1, op=op), reads, writes)

    def ts(self, eng, out, in0, s1, s2, op0, op1, reads, writes):
        if op1 is None:
            self.p.add(eng, lambda e: e.tensor_scalar(out=out, in0=in0, scalar1=s1, scalar2=None, op0=op0), reads, writes)
        else:
            self.p.add(eng, lambda e: e.tensor_scalar(out=out, in0=in0, scalar1=s1, scalar2=s2, op0=op0, op1=op1), reads, writes)

    def stt(self, out, in0, scalar, in1, op0, op1, reads, writes):
        self.p.add("dve", lambda e: e.scalar_tensor_tensor(out=out, in0=in0, scalar=scalar, in1=in1, op0=op0, op1=op1), reads, writes)

    def cp(self, eng, out, in_, reads, writes):
        if eng == "act":
            self.act(out, in_, AF.Copy, reads, writes)
        else:
            self.p.add(eng, lambda e: e.tensor_copy(out=out, in_=in_), reads, writes)

    def recip_act(self, out, in_, reads, writes):
        self.act(out, in_, AF.Ln, reads, writes)
        self.act(out, out, AF.Exp, writes, writes, scale=-1.0)

    def rsqrt_act(self, out, in_, scale, reads, writes):
        self.act(out, in_, AF.Ln, reads, writes, scale=scale, bias=self.epsb[:, 0:1])
        self.act(out, out, AF.Exp, writes, writes, scale=-0.5)

    def recip(self, out, in_, reads, writes):
        self.p.add("dve", lambda e: e.reciprocal(out=out, in_=in_), reads, writes)

    def scan(self, out, d0, d1, init, reads, writes):
        self.p.add("dve", lambda e: e.tensor_tensor_scan(out=out, data0=d0, data1=d1, initial=init, op0=ALU.mult, op1=ALU.add), reads, writes)

    def memset(self, eng, ap, val, reads, writes):
        self.p.add(eng, lambda e: e.memset(ap, val), reads, writes)

    def dma(self, q, out, in_, reads, writes):
        self.p.add(q, lambda e: e.dma_start(out=out, in_=in_), reads, writes, dma=True)

    def join(self, key_write, extra_reads=()):
        d = self.dummy
        self.p.add("pool", lambda e: e.memset(d[:], 0.0), list(extra_reads) + ["dummy_r"], [key_write, "dummy"])

    def bank(self, hold=False):
        while True:
            i = self.psrot % 8
            self.psrot += 1
            if i not in self.held:
                break
        if hold:
            self.held.add(i)
        return self.ps[i], ("ps", i)

    def release(self, *keys):
        for k in keys:
            self.held.discard(k[1])

    def tap(self, name, ap, shape, dt, reads):
        if name not in self.taps:
            return
        if "tap_" + name not in self.tap_specs:
            self.tap_specs["tap_" + name] = self.dram_out("tap_" + name, shape, dt)
        t = self.tap_specs["tap_" + name]
        self.dma("sp", t, ap, reads, ["tapout_" + name])

    def wplan_build(self):
        self.wplan = []
        for li, l in enumerate(self.layers):
            win, wo, w1, w2 = self.w["w_in"][l], self.w["w_out"][l], self.w["mlp_w1"][l], self.w["mlp_w2"][l]
            for s in range(self.nseq):
                for n in range(self.ntiles):
                    ft = (s == 0 and n == 0)
                    for b in range(6):
                        ncols = 512 if b < 5 else 256
                        self.wplan.append((win[:, b * 512:b * 512 + ncols], b, ncols, ft))
                    for h in range(2):
                        self.wplan.append((wo[:, h * 512:(h + 1) * 512], 6 + h, 512, ft))
                    for b in range(8):
                        self.wplan.append((w1[:, b * 512:(b + 1) * 512], 8 + b, 512, ft))
                    for h in range(2):
                        for q in range(4):
                            self.wplan.append((w2[q * 1024:(q + 1) * 1024, h * 512:(h + 1) * 512], 16 + 4 * h + q, 512, ft))
        self.wptr = 0
        self.wissued = 0

    def _wissue(self, i):
        src_ap, blk, ncols, ft = self.wplan[i]
        buf = self.wpool[i % NW]
        key = ("w", i % NW)
        scr = self.wscr[blk][:, 0:8 * ncols].rearrange("p (k c) -> p k c", c=ncols)
        if ft:
            self.dma("pool", buf[:, :, 0:ncols], src_ap.rearrange("(kt p) c -> p kt c", p=128), [], [key])
            self.dma("sp", scr, buf[:, :, 0:ncols], [key], [("wscr", blk)])
        else:
            self.dma("pool", buf[:, :, 0:ncols], scr, [("wscr", blk)], [key])

    def wblock(self, blk, look=2):
        i = self.wptr
        assert self.wplan[i][1] == blk, (i, self.wplan[i][1], blk)
        lim = min(len(self.wplan), i + look + 1)
        while self.wissued < lim:
            self._wissue(self.wissued)
            self.wissued += 1
        self.wptr += 1
        return self.wpool[i % NW], ("w", i % NW)

    def build(self):
        nc = self.nc
        nseq, ntiles = self.nseq, self.ntiles
        ntok = nseq * SEQ
        self.x_in = self.dram_in("x", [ntok, D], F32)
        self.out = self.dram_out("out", [ntok, D], F32)
        self.w = {}
        for name, shape in WEIGHT_SPECS:
            self.w[name] = self.dram_in(name, shape, F32)
        self.prm = {}
        for name, shape, dt in PARAM_SPECS:
            self.prm[name] = self.dram_in(name, shape, dt)
        self.cst_d = {}
        for name, shape, dt in CONST_SPECS:
            self.cst_d[name] = self.dram_in("c_" + name, shape, dt)
        self.xmid = nc.dram_tensor("xmid", [ntok, D], F32).ap()
        self.vscr = [nc.dram_tensor("vscr%d" % i, [T, 512], BF16).ap() for i in range(2)]
        self.wscr = nc.dram_tensor("wscr", [24, 128, 4096], BF16).ap()

        sb = self.sb
        self.cst = {}
        for name, shape, dt in CONST_SPECS:
            self.cst[name] = sb("k_" + name, shape, dt)
        self.ident = self.cst["ident"]
        self.dummy = sb("dmy0", [128, 8], F32)
        self.xtok = sb("xtok", [128, 4, D], F32)
        self.xT = sb("xT", [128, 8, T], BF16)
        self.khist = sb("khist", [128, 4, SEQ], BF16)
        self.v3 = sb("v3", [128, 16, 512], BF16)
        self.v1 = sb("v1", [128, 5, 512], BF16)
        self.v4 = sb("v4", [128, 2, 4, 512], BF16)
        self.wpool = [sb("wp%d" % i, [128, 8, 512], BF16) for i in range(NW)]
        self.lnp = sb("lnp", [128, 2, D], F32)
        self.btb = sb("btb", [128, 8, 2, 128], BF16)
        self.ctb = sb("ctb", [128, 8, 2, 128], BF16)
        self.tabc = sb("tabc", [128, 8, 128], F32)
        self.tabs = sb("tabs", [128, 8, 2, 128], F32)
        self.s5p = sb("s5p", [128, 20, 8], F32)
        self.vecs = sb("vecs", [128, 12], F32)
        self.wglu = sb("wglu", [128, 2, 256], BF16)
        self.diagd = sb("diagd", [128, 2, 128], BF16)
        self.carry = sb("carry", [128, 2, 8], F32)
        self.wl = sb("wl", [128, 2, 8], F32)
        self.r32 = sb("r32", [128, 2, 64], F32)
        self.rbf = sb("rbf", [128, 2, 64], BF16)
        self.lns = sb("lns", [128, 2, 8], F32)
        self.bnst = sb("bnst", [128, 2, 2, 6], F32)
        self.regA = sb("regA", [128, 32 * T], BF16)
        rA = self.regA
        self.hidden = rA[:, :].rearrange("p (k t) -> p k t", t=T)
        self.zT = rA[:, 0:12 * T].rearrange("p (k t) -> p k t", t=T)
        o = 12 * T
        self.rkz = rA[:, o:o + 1024].rearrange("p (c d) -> p c d", c=4)
        o += 1024
        self.rvt = rA[:, o:o + 1024].rearrange("p (c d) -> p c d", c=4)
        o += 1024
        self.rqx = rA[:, o:o + 2 * T].rearrange("p (k t) -> p k t", t=T)
        o += 2 * T
        def f32view(n):
            nonlocal o
            v = rA[:, o:o + 2 * n].bitcast(F32)
            o += 2 * n
            return v
        self.s5t1 = [f32view(256) for _ in range(2)]
        self.s5t2 = [f32view(256) for _ in range(2)]
        self.s5w = [f32view(384) for _ in range(4)]
        self.s5ta = [f32view(256) for _ in range(2)]
        self.s5x = []
        for _ in range(4):
            self.s5x.append(rA[:, o:o + 256])
            o += 256
        assert o <= 32 * T, o
        self.yT = sb("yT", [128, 8, T], BF16)
        self.fs = [sb("fs%d" % i, [128, T], F32) for i in range(6)]
        self.bs = [sb("bs%d" % i, [128, T], BF16) for i in range(8)]
        self.xbn = sb("xbn", [128, 4, D], BF16)
        self.xb = sb("xb", [128, D], BF16)
        self.ps = [self.stack.enter_context(nc.psum_tensor("ps%d" % i, [128, 512], F32)) for i in range(8)]

        for name, shape, dt in CONST_SPECS:
            self.dma("sp", self.cst[name][:], self.cst_d[name], [], ["const"])
        self.memset("pool", self.dummy[:], 0.0, [], ["dummy"])

        self.wplan_build()
        self.tiles = [(li, l, s, n) for li, l in enumerate(self.layers) for s in range(nseq) for n in range(ntiles)]
        self.prefetch_x(0)
        for ti, (li, l, s, n) in enumerate(self.tiles):
            first = li == 0
            last = li == len(self.layers) - 1
            if s == 0 and n == 0:
                self.layer_setup(l)
            if n == 0:
                self.seq_reset()
            self.ti = ti
            self.tile(l, s, n, first, last)
        self.p.emit(nc, self.stack)
        return nc

    def layer_setup(self, l):
        P = self.prm
        s5p = self.s5p
        rk = ["s5prm"]
        self.dma("sp", s5p[:, 0:3, :], P["lam"][l], [], rk)
        self.dma("sp", self.vecs[:], P["vecs"][l], [], ["vecs"])
        self.dma("pool", self.btb[:], P["bt"][l], [], ["btb"])
        self.dma("pool", self.wglu[:], self.w["ssm_w_glu"][l].rearrange("(kt p) c -> p kt c", p=128), [], ["wglu"])
        ct_st = [self.fs[i] for i in range(4)]
        for i in range(4):
            self.dma("sp", ct_st[i][:, :].rearrange("p (g r c) -> p g r c", g=2, r=2),
                     P["ct"][l][:, 2 * i:2 * i + 2], [("fs", i)], [("fs", i)])
        lr, li_, ldt = s5p[:, 0, :], s5p[:, 1, :], s5p[:, 2, :]
        dt_, th, mag = s5p[:, 3, :], s5p[:, 4, :], s5p[:, 5, :]
        cc, ss = s5p[:, 6, :], s5p[:, 7, :]
        t1, t2, t3 = s5p[:, 8, :], s5p[:, 9, :], s5p[:, 10, :]
        fre, fim = s5p[:, 11, :], s5p[:, 12, :]
        are, aim = s5p[:, 13, :], s5p[:, 14, :]
        den = s5p[:, 15, :]
        A = lambda out, in_, f, **kw: self.act(out, in_, f, rk, rk, **kw)
        V = lambda out, a, b, op: self.tt("dve", out, a, b, op, rk, rk)
        A(dt_, ldt, AF.Exp)
        V(t1, lr, dt_, ALU.mult)
        A(mag, t1, AF.Exp)
        V(th, li_, dt_, ALU.mult)
        A(ss, th, AF.Sin, scale=1.0 / 16)
        self.ts("dve", t2, th, 1.0 / 16, math.pi / 2, ALU.mult, ALU.add, rk, rk)
        A(cc, t2, AF.Sin)
        for _ in range(4):
            V(t1, cc, cc, ALU.mult)
            V(t2, ss, ss, ALU.mult)
            V(t3, cc, ss, ALU.mult)
            V(cc, t1, t2, ALU.subtract)
            self.ts("dve", ss, t3, 2.0, None, ALU.mult, None, rk, rk)
        V(are, mag, cc, ALU.mult)
        V(aim, mag, ss, ALU.mult)
        V(t1, lr, lr, ALU.mult)
        V(t2, li_, li_, ALU.mult)
        V(den, t1, t2, ALU.add)
        self.recip(den, den, rk, rk)
        self.ts("dve", t3, are, -1.0, None, ALU.add, None, rk, rk)
        V(t1, t3, lr, ALU.mult)
        V(t2, aim, li_, ALU.mult)
        V(t1, t1, t2, ALU.add)
        V(fre, t1, den, ALU.mult)
        V(t1, aim, lr, ALU.mult)
        V(t2, t3, li_, ALU.mult)
        V(t1, t1, t2, ALU.subtract)
        V(fim, t1, den, ALU.mult)
        for gp in range(8):
            st = ct_st[gp // 2][:, :].rearrange("p (g r c) -> p g r c", g=2, r=2)
            cre, cim = st[:, gp % 2, 0, :], st[:, gp % 2, 1, :]
            fk = ("fs", gp // 2)
            tmpa = self.fs[4][:, 0:128]
            tmpb = self.fs[4][:, 128:256]
            tk = [("fs", 4)]
            self.ts("dve", tmpa, cre, fre[:, gp:gp + 1], None, ALU.mult, None, rk + [fk], tk)
            self.stt(tmpb, cim, fim[:, gp:gp + 1], tmpa, ALU.mult, ALU.subtract, rk + [fk] + tk, tk)
            self.ts("dve", self.ctb[:, gp, 0, :], tmpb, -1.0, None, ALU.mult, None, tk, ["ctb"])
            self.ts("dve", tmpa, cre, fim[:, gp:gp + 1], None, ALU.mult, None, rk + [fk], tk)
            self.stt(tmpb, cim, fre[:, gp:gp + 1], tmpa, ALU.mult, ALU.add, rk + [fk] + tk, tk)
            self.ts("dve", self.ctb[:, gp, 1, :], tmpb, -1.0, None, ALU.mult, None, tk, ["ctb"])
        tc, tsn = self.tabc, self.tabs
        tk = ["tab"]
        self.cp("dve", tc[:, :, 0], cc, rk, tk)
        self.cp("dve", tsn[:, :, 0, 0], ss, rk, tk)
        pc, psn = s5p[:, 8, :], s5p[:, 9, :]
        self.cp("dve", pc, cc, rk, rk)
        self.cp("dve", psn, ss, rk, rk)
        q1, q2, q3 = s5p[:, 10, :], s5p[:, 13, :], s5p[:, 14, :]
        Lc = 1
        tmpA = self.fs[4][:, :].rearrange("p (g j) -> p g j", g=8)
        tmpB = self.fs[5][:, :].rearrange("p (g j) -> p g j", g=8)
        while Lc < 128:
            pcb = pc.unsqueeze(2).to_broadcast([128, 8, Lc])
            psb = psn.unsqueeze(2).to_broadcast([128, 8, Lc])
            c0, s0 = tc[:, :, 0:Lc], tsn[:, :, 0, 0:Lc]
            c1, s1 = tc[:, :, Lc:2 * Lc], tsn[:, :, 0, Lc:2 * Lc]
            ta, tb = tmpA[:, :, 0:Lc], tmpB[:, :, 0:Lc]
            k4, k5 = [("fs", 4)], [("fs", 5)]
            self.tt("dve", ta, c0, pcb, ALU.mult, rk + tk, k4)
            self.tt("dve", tb, s0, psb, ALU.mult, rk + tk, k5)
            self.tt("dve", c1, ta, tb, ALU.subtract, k4 + k5, tk)
            self.tt("dve", ta, c0, psb, ALU.mult, rk + tk, k4)
            self.tt("dve", tb, s0, pcb, ALU.mult, rk + tk, k5)
            self.tt("dve", s1, ta, tb, ALU.add, k4 + k5, tk)
            V(q1, pc, pc, ALU.mult)
            V(q2, psn, psn, ALU.mult)
            V(q3, pc, psn, ALU.mult)
            V(pc, q1, q2, ALU.subtract)
            self.ts("dve", psn, q3, 2.0, None, ALU.mult, None, rk, rk)
            Lc *= 2
        self.ts("dve", tsn[:, :, 1, :], tsn[:, :, 0, :], -1.0, None, ALU.mult, None, tk, tk)
        for h in range(2):
            self.ts("dve", self.diagd[:, h, :], self.ident[:], self.vecs[:, h:h + 1], None, ALU.mult, None,
                    ["const", "vecs"], ["diagd"])

    def prefetch_x(self, ti):
        if ti >= len(self.tiles):
            return
        li, l, s, n = self.tiles[ti]
        row0 = s * SEQ + n * T
        src = self.x_in if li == 0 else self.xmid
        rd = [] if li == 0 else [("xmid", s, n)]
        self.dma("pool", self.xbn[:], src[row0:row0 + T, :].rearrange("(c p) d -> p c d", p=128), rd, ["xbn"])

    def seq_reset(self):
        self.memset("dve", self.carry[:], 0.0, [], ["carry"])
        self.memset("dve", self.r32[:], 0.0, [], ["r32"])
        self.memset("pool", self.rbf[:], 0.0, [], ["rbf"])

    def tile(self, l, s, n, first, last):
        ph = self.phases
        row0 = s * SEQ + n * T
        gA = ["gA", "gB"]
        src = self.x_in if first else self.xmid
        rd = [] if first else [("xmid", s, n)]
        self.dma("sp", self.xtok[:], src[row0:row0 + T, :].rearrange("(c p) d -> p c d", p=128), rd,
                 [("xtok", c) for c in range(4)])
        self.join("gB")
        for c in range(4):
            self.xT_chunk(c, self.xbn[:, c, :], ["xbn"])
        self.prefetch_x(self.ti + 1)
        self.phase_win(l, s, n)
        self.tap("zT", self.zT, [128, 12, T], BF16, gA + [("zT", j) for j in range(12)])
        self.tap("rkz", self.rkz, [128, 4, 256], BF16, gA + ["rkz"])
        self.tap("rvt", self.rvt, [128, 4, 256], BF16, gA + ["rvt"])
        self.tap("v1", self.v1[:, 0:4, :], [128, 4, 512], BF16, [("V1", c) for c in range(4)])
        if ph is None:
            g1 = self.s5_main_gen(l, s, n)
            g2 = self._chain(self.ret_gen(l, s, n), self.att_gen(l, s, n))
            alive1 = alive2 = True
            cyc = 0
            while alive1 or alive2:
                if alive1:
                    alive1 = next(g1, "end") != "end"
                for _ in range(3 if cyc % 2 == 0 else 2):
                    if alive2:
                        alive2 = next(g2, "end") != "end"
                cyc += 1
            self.s5_post(l, s, n)
        else:
            if "s5" in ph:
                self.phase_s5(l, s, n)
            if "ret" in ph:
                self.phase_ret(l, s, n)
            if "att" in ph:
                self.phase_att(l, s, n)
        self.tap("yT", self.yT[:], [128, 8, T], BF16, [("yT", j) for j in range(8)])
        if ph is None or "out" in ph:
            self.phase_outproj(l, s, n)
            self.tap("x1", self.xtok[:], [128, 4, D], F32, [("xtok", c) for c in range(4)])
        if ph is None or "mlp" in ph:
            self.join("gA")
            self.phase_mlp(l, s, n)
        dst = self.out if last else self.xmid
        wr = ["outdram"] if last else [("xmid", s, n)]
        self.dma("sp", dst[row0:row0 + T, :].rearrange("(c p) d -> p c d", p=128), self.xtok[:],
                 [("xtok", c) for c in range(4)], wr)

    @staticmethod
    def _chain(*gens):
        for g in gens:
            for _ in g:
                yield

    def xT_chunk(self, c, src_bf, src_keys):
        bk, bkey = self.bank()
        pb = bk[:].bitcast(BF16)
        for kt in range(8):
            self.tr(pb[:, kt * 128:(kt + 1) * 128], src_bf[:, kt * 128:(kt + 1) * 128], src_keys, [bkey])
        self.cp("act", self.xT[:, :, c * 128:(c + 1) * 128], pb[:, :].rearrange("p (k t) -> p k t", k=8),
                [bkey], [("xT", c)])

    def phase_win(self, l, s, n):
        gA = ["gA", "gB"]
        xTk = [("xT", c) for c in range(4)]
        win = self.w["w_in"][l]
        par = n % 2
        for b in range(6):
            ncols = 512 if b < 5 else 256
            buf, wkey = self.wblock(b)
            for jj in range(ncols // 128):
                j = 4 * b + jj
                wc = slice(jj * 128, (jj + 1) * 128)
                fm = None
                if j < 6:
                    fm = (self.zT[:, j, :], [("zT", j)], True)
                elif 8 <= j < 14:
                    fm = (self.zT[:, j - 2, :], [("zT", j - 2)], True)
                elif 14 <= j < 18:
                    fm = (self.khist[:, j - 14, n * T:(n + 1) * T], [("K", j - 14, n)], False)
                if fm is not None:
                    bk, bkey = self.bank()
                    for kt in range(8):
                        self.mm(bk[:, :], buf[:, kt, wc], self.xT[:, kt, :], kt == 0, kt == 7,
                                [wkey] + xTk, [bkey])
                    dest, wkeys, inA = fm
                    self.cp("act", dest, bk[:, :], [bkey] + (gA if inA else []), wkeys)
                tm = 4 <= j < 8 or j >= 18
                if tm:
                    bk, bkey = self.bank()
                    for c in range(4):
                        for kt in range(8):
                            self.mm(bk[:, c * 128:(c + 1) * 128], self.xT[:, kt, c * 128:(c + 1) * 128], buf[:, kt, wc],
                                    kt == 0, kt == 7, [wkey, ("xT", c)], [bkey])
                    if j < 6:
                        h0 = 2 * (j - 4)
                        for c in range(4):
                            self.tt("dve", self.rkz[:, c, (j - 4) * 128:(j - 3) * 128].rearrange("p (h d) -> p h d", h=2),
                                    bk[:, c * 128:(c + 1) * 128].rearrange("p (h d) -> p h d", h=2),
                                    self.cst["zeta"][:, h0:h0 + 2].unsqueeze(2).to_broadcast([128, 2, 64]),
                                    ALU.mult, [bkey, "const"] + gA, ["rkz"])
                    elif j < 8:
                        self.cp("act", self.rvt[:, :, (j - 6) * 128:(j - 5) * 128],
                                bk[:, :].rearrange("p (c d) -> p c d", c=4), [bkey] + gA, ["rvt"])
                    else:
                        self.cp("act", self.v1[:, 0:4, (j - 18) * 128:(j - 17) * 128],
                                bk[:, :].rearrange("p (c d) -> p c d", c=4), [bkey], [("V1", c) for c in range(4)])
        vs = self.vscr[par]
        v1k = [("V1", c) for c in range(4)]
        self.dma("sp", vs.rearrange("(c p) d -> p c d", p=128), self.v1[:, 0:4, :], v1k, [("vscr", par)])
        self.dma("sp", self.v4[:, par, :, :], vs.rearrange("(i r) d -> i r d", r=4), [("vscr", par)], [("V4", par)])
        self.dma("sp", self.v3[32 * n:32 * n + 32, :, :], vs.rearrange("(i r) d -> i r d", r=16), [("vscr", par)],
                 [("V3", n)])

    def s5_main_gen(self, l, s, n):
        gA = ["gA", "gB"]
        uT = [self.zT[:, 0, :], self.zT[:, 1, :]]
        ybk = [self.bank(hold=True), self.bank(hold=True)]
        self.s5_ybk = ybk
        mag = self.s5p[:, 5, :]
        v3d = lambda ap: ap.rearrange("p (a b) -> p a b", a=2)
        pending = []
        for c in range(4):
            cs = slice(c * 128, (c + 1) * 128)
            for h in range(2):
                yb, ykey = ybk[h]
                self.mm(yb[:, cs], self.diagd[:, h, :], uT[h][:, cs], True, False, ["diagd", ("zT", h)] + gA, [ykey])
            for gp0 in (0, 2, 4, 6):
                pair = (gp0, gp0 + 1)
                h = gp0 // 4
                pp = self.s5pp % 2
                self.s5pp += 1
                st = {}
                for gp in pair:
                    g = gp % 2
                    j = pp * 2 + g
                    bk, bkey = self.bank()
                    rd = ["btb", ("zT", h)] + gA
                    self.mm(bk[:, 0:128], self.btb[:, gp, 0, :], uT[h][:, cs], True, True, rd, [bkey])
                    self.mm(bk[:, 128:256], self.btb[:, gp, 1, :], uT[h][:, cs], True, True, rd, [bkey])
                    self.mm(bk[:, 256:384], self.btb[:, gp, 0, :], uT[h][:, cs], True, True, rd, [bkey])
                    st[gp] = dict(bk=bk, bkey=bkey, t1=self.s5t1[g], t2=self.s5t2[g], w=self.s5w[j], ta=self.s5ta[g],
                                  x=self.s5x[j], k1=("s5t1", g), k2=("s5t2", g), kw=("s5w", j), ka=("s5ta", g),
                                  kx=("s5x", j), cb=self.tabc[:, gp, :].unsqueeze(1).to_broadcast([128, 2, 128]),
                                  sbb=self.tabs[:, gp, :, :], magb=mag[:, gp:gp + 1].to_broadcast([128, 128]))
                while pending:
                    pending.pop(0)()
                for gp in pair:
                    d = st[gp]
                    self.tt("dve", v3d(d["t1"][:, :]), v3d(d["bk"][:, 0:256]), d["cb"], ALU.mult, [d["bkey"], "tab"] + gA, [d["k1"]])
                    self.tt("dve", v3d(d["t2"][:, :]), v3d(d["bk"][:, 128:384]), d["sbb"], ALU.mult, [d["bkey"], "tab"] + gA, [d["k2"]])
                for gp in pair:
                    d = st[gp]
                    self.tt("dve", d["t1"][:, :], d["t1"][:, :], d["t2"][:, :], ALU.add, [d["k1"], d["k2"]] + gA, [d["k1"]])
                for gp in pair:
                    d = st[gp]
                    w, v = d["w"], d["t1"]
                    self.scan(w[:, 0:128], d["magb"], v[:, 0:128], self.carry[:, 0, gp:gp + 1], [d["k1"], "carry", "s5prm"] + gA, [d["kw"]])
                    self.scan(w[:, 128:256], d["magb"], v[:, 128:256], self.carry[:, 1, gp:gp + 1], [d["k1"], "carry", "s5prm"] + gA, [d["kw"]])
                for gp in pair:
                    d = st[gp]
                    w = d["w"]
                    self.cp("act", w[:, 256:384], w[:, 0:128], [d["kw"]] + gA, [d["kw"]])
                    self.cp("act", self.wl[:, :, gp], w[:, 127:256:128], [d["kw"]] + gA, ["wl"])
                for gp in pair:
                    d = st[gp]
                    self.tt(OUTROT_ENG, v3d(d["ta"][:, :]), v3d(d["w"][:, 0:256]), d["cb"], ALU.mult, [d["kw"], "tab"] + gA, [d["ka"]])
                for gp in pair:
                    d = st[gp]
                    self.tt(OUTROT_ENG, v3d(d["w"][:, 128:384]), v3d(d["w"][:, 128:384]), d["sbb"], ALU.mult, [d["kw"], "tab"] + gA, [d["kw"]])
                for gp in pair:
                    d = st[gp]
                    self.tt(OUTROT_ENG, d["x"][:, :], d["ta"][:, :], d["w"][:, 128:384], ALU.subtract, [d["ka"], d["kw"]] + gA, [d["kx"]])
                def cmm(pair=pair, st=st, h=h, cs=cs):
                    for gp in pair:
                        d = st[gp]
                        yb, ykey = ybk[h]
                        lastg = gp % 4 == 3
                        self.mm(yb[:, cs], self.ctb[:, gp, 0, :], d["x"][:, 0:128], False, False, ["ctb", d["kx"]] + gA, [ykey])
                        self.mm(yb[:, cs], self.ctb[:, gp, 1, :], d["x"][:, 128:256], False, lastg, ["ctb", d["kx"]] + gA, [ykey])
                pending.append(cmm)
                yield
            while pending:
                pending.pop(0)()
            cl = self.tabc[:, :, 127]
            sl = self.tabs[:, :, 0, 127]
            wr_, wi_ = self.wl[:, 0, :], self.wl[:, 1, :]
            a_, b_ = self.s5p[:, 16, :], self.s5p[:, 17, :]
            c_, d_ = self.s5p[:, 18, :], self.s5p[:, 19, :]
            self.tt("dve", a_, cl, wr_, ALU.mult, ["tab", "wl"], ["s5tmpa"])
            self.tt("dve", b_, sl, wi_, ALU.mult, ["tab", "wl"], ["s5tmpb"])
            self.tt("dve", c_, cl, wi_, ALU.mult, ["tab", "wl"], ["s5tmpc"])
            self.tt("dve", d_, sl, wr_, ALU.mult, ["tab", "wl"], ["s5tmpd"])
            self.tt("dve", self.carry[:, 0, :], a_, b_, ALU.subtract, ["s5tmpa", "s5tmpb"], ["carry"])
            self.tt("dve", self.carry[:, 1, :], c_, d_, ALU.add, ["s5tmpc", "s5tmpd"], ["carry"])
            yield

    def phase_s5(self, l, s, n):
        for _ in self.s5_main_gen(l, s, n):
            pass
        self.s5_post(l, s, n)

    def s5_post(self, l, s, n):
        ybk = self.s5_ybk
        g32 = [self.fs[0], self.fs[1]]
        gbf = [self.bs[0], self.bs[1]]
        for h in range(2):
            yb, ykey = ybk[h]
            self.act(g32[h][:, :], yb[:, :], AF.Gelu_apprx_tanh, [ykey], [("fs", h)])
            self.act(gbf[h][:, :], yb[:, :], AF.Gelu_apprx_tanh, [ykey], [("bs", h)])
        self.release(ybk[0][1], ybk[1][1])
        self.tap("s5g", self.fs[0][:, :], [128, T], F32, [("fs", 0)])
        o32 = [self.fs[2], self.fs[3]]
        sqb = [self.bs[2], self.bs[3]]
        for m in range(2):
            bk, bkey = self.bank()
            for kt in range(2):
                self.mm(bk[:, :], self.wglu[:, kt, m * 128:(m + 1) * 128], gbf[kt][:, :], kt == 0, kt == 1,
                        ["wglu", ("bs", kt)], [bkey])
            self.act(o32[m][:, :], bk[:, :], AF.Sigmoid, [bkey, "vecs"], [("fs", 2 + m)], bias=self.vecs[:, 2 + m:3 + m])
            self.tt("dve", o32[m][:, :], o32[m][:, :], g32[m][:, :], ALU.mult, [("fs", 2 + m), ("fs", m)], [("fs", 2 + m)])
            self.act(sqb[m][:, :], o32[m][:, :], AF.Square, [("fs", 2 + m)], [("bs", 2 + m)])
        bk, bkey = self.bank()
        for m in range(2):
            self.mm(bk[:, :], self.cst["ones"][:], sqb[m][:, :], m == 0, m == 1, ["const", ("bs", 2 + m)], [bkey])
        rs = self.fs[4]
        self.rsqrt_act(rs[:, :], bk[:, :], 1.0 / 256, [bkey], [("fs", 4)])
        for m in range(2):
            self.stt(self.yT[:, m, :], o32[m][:, :], self.vecs[:, 4 + m:5 + m], rs[:, :], ALU.mult, ALU.mult,
                     [("fs", 2 + m), ("fs", 4), "vecs"], [("yT", m)])

    def phase_ret(self, l, s, n):
        for _ in self.ret_gen(l, s, n):
            pass

    def ret_gen(self, l, s, n):
        gA = ["gA", "gB"]
        rqT = [self.zT[:, 2, :], self.zT[:, 3, :]]
        rkT = [self.zT[:, 4, :], self.zT[:, 5, :]]
        rgT = [self.zT[:, 6, :], self.zT[:, 7, :]]
        C = self.cst
        for hp in range(2):
            self.tt("dve", self.rqx[:, hp, :].rearrange("p (c q) -> p c q", c=4),
                    rqT[hp].rearrange("p (c q) -> p c q", c=4),
                    C["xi"][:, hp, :].unsqueeze(1).to_broadcast([128, 4, 128]), ALU.mult,
                    [("zT", 2 + hp), "const"] + gA, [("rqx", hp)])
        obk = [self.bank(hold=True), self.bank(hold=True)]

        def st_a(c):
            cs = slice(c * 128, (c + 1) * 128)
            sb2 = [self.bank(), self.bank()]
            for h in range(4):
                hp, h2 = h // 2, h % 2
                pr = slice(64 * h2, 64 * h2 + 64)
                sbk, skey = sb2[h2]
                self.mm(sbk[:, hp * 128:(hp + 1) * 128], rkT[hp][pr, cs], rqT[hp][pr, cs], True, True,
                        [("zT", 4 + hp), ("zT", 2 + hp)] + gA, [skey])
            pt = self.bs[4 + (c % 2)]
            pk = ("bs", 4 + (c % 2))
            for h2 in range(2):
                sbk, skey = sb2[h2]
                self.tt("dve", pt[:, h2 * 256:(h2 + 1) * 256], sbk[:, 0:256],
                        C["dr"][:, h2].rearrange("p a q -> p (a q)"), ALU.mult, [skey, "const", pk], [pk])

        def st_b(c):
            cs = slice(c * 128, (c + 1) * 128)
            pt = self.bs[4 + (c % 2)]
            pk = ("bs", 4 + (c % 2))
            for h in range(4):
                hp, h2 = h // 2, h % 2
                pr = slice(64 * h2, 64 * h2 + 64)
                ob, okey = obk[hp]
                pcol = slice(h2 * 256 + hp * 128, h2 * 256 + hp * 128 + 128)
                self.mm(ob[pr, cs], self.rvt[:, c, h * 64:(h + 1) * 64], pt[:, pcol], True, False,
                        ["rvt", pk] + gA, [okey])
                self.mm(ob[pr, cs], self.rbf[pr, hp, :], self.rqx[pr, hp, cs], False, True,
                        ["rbf", ("rqx", hp)] + gA, [okey], tile_position=(64 * h2, 64 * h2))

        def st_u(c):
            kvb, kvkey = self.bank()
            for h in range(4):
                hp, h2 = h // 2, h % 2
                pr = slice(64 * h2, 64 * h2 + 64)
                self.mm(kvb[pr, hp * 64:(hp + 1) * 64], self.rkz[:, c, h * 64:(h + 1) * 64], self.rvt[:, c, h * 64:(h + 1) * 64],
                        True, True, ["rkz", "rvt"] + gA, [kvkey])
            for hp in range(2):
                self.stt(self.r32[:, hp, :], self.r32[:, hp, :], C["gch"][:, hp:hp + 1], kvb[:, hp * 64:(hp + 1) * 64],
                         ALU.mult, ALU.add, ["r32", kvkey, "const"], ["r32"])
            self.cp("act", self.rbf[:], self.r32[:], ["r32"], ["rbf"])

        st_a(0)
        for c in range(4):
            if c + 1 < 4:
                st_a(c + 1)
                yield
            st_b(c)
            st_u(c)
            yield
        for hp in range(2):
            ob, okey = obk[hp]
            o32, obf, osq = self.fs[0], self.bs[0], self.bs[1]
            self.cp("act", o32[:, :], ob[:, :], [okey], [("fs", 0)])
            self.cp("act", obf[:, :], ob[:, :], [okey], [("bs", 0)])
            self.act(osq[:, :], ob[:, :], AF.Square, [okey], [("bs", 1)])
            mb, mkey = self.bank()
            self.mm(mb[:, :], C["blk64"][:], obf[:, :], True, True, ["const", ("bs", 0)], [mkey])
            eb, ekey = self.bank()
            self.mm(eb[:, :], C["blk64"][:], osq[:, :], True, True, ["const", ("bs", 1)], [ekey])
            mean, var = self.fs[1], self.fs[2]
            self.cp("act", mean[:, :], mb[:, :], [mkey], [("fs", 1)])
            self.tt("dve", var[:, :], mean[:, :], mean[:, :], ALU.mult, [("fs", 1)], [("fs", 2)])
            self.tt("dve", var[:, :], eb[:, :], var[:, :], ALU.subtract, [ekey, ("fs", 2)], [("fs", 2)])
            self.ts("dve", var[:, :], var[:, :], 0.0, None, ALU.max, None, [("fs", 2)], [("fs", 2)])
            self.rsqrt_act(var[:, :], var[:, :], 1.0, [("fs", 2)], [("fs", 2)])
            self.tt("dve", o32[:, :], o32[:, :], mean[:, :], ALU.subtract, [("fs", 0), ("fs", 1)], [("fs", 0)])
            self.tt("dve", o32[:, :], o32[:, :], var[:, :], ALU.mult, [("fs", 0), ("fs", 2)], [("fs", 0)])
            sg = self.fs[3]
            self.act(sg[:, :], rgT[hp], AF.Silu, [("zT", 6 + hp)] + gA, [("fs", 3)])
            self.stt(self.yT[:, 2 + hp, :], o32[:, :], self.vecs[:, 6 + hp:7 + hp], sg[:, :], ALU.mult, ALU.mult,
                     [("fs", 0), ("fs", 3), "vecs"], [("yT", 2 + hp)])
            yield
        self.release(obk[0][1], obk[1][1])

    def phase_att(self, l, s, n):
        for _ in self.att_gen(l, s, n):
            pass

    def att_gen(self, l, s, n):
        gA = ["gA", "gB"]
        C = self.cst
        ones64 = C["ones"][:, 0:64]
        self.nE = getattr(self, "nE", 0)
        o32 = [self.fs[i] for i in range(4)]
        for hp in range(4):
            aq = self.zT[:, 8 + hp, :]
            aqk = [("zT", 8 + hp)] + gA
            ob, okey = self.bank(hold=True)
            lb, lkey = self.bank(hold=True)
            started = [False, False]

            def pv(vrows, pt, pk, item_cols, out_cols, ksz, h2, vkeys, last=False):
                pr = slice(64 * h2, 64 * h2 + 64)
                st = not started[h2]
                started[h2] = True
                self.mm(ob[pr, out_cols], vrows, pt[0:ksz, item_cols], st, last,
                        list(vkeys) + [pk], [okey], tile_position=(0, 64 * h2))
                self.mm(lb[pr, out_cols], ones64[0:ksz, :], pt[0:ksz, item_cols], st, last,
                        ["const", pk], [lkey], tile_position=(0, 64 * h2))

            def hcol(h2):
                return slice((2 * hp + h2) * 64, (2 * hp + h2) * 64 + 64)

            def scratch():
                i = self.nE % 2
                self.nE += 1
                E = [(self.bs[4 * i + 0], ("bs", 4 * i + 0)), (self.bs[4 * i + 1], ("bs", 4 * i + 1))]
                Pb = [(self.bs[4 * i + 2], ("bs", 4 * i + 2)), (self.bs[4 * i + 3], ("bs", 4 * i + 3))]
                return E, Pb

            units = []

            def make_unit12(br, pair):
                dtab = C["d1"] if br == 1 else C["d2"]
                us = [2 * pair, 2 * pair + 1]
                valid = []
                for ui, u in enumerate(us):
                    for rel in (0, 1):
                        if br == 1 and 4 * n + u - rel < 0:
                            continue
                        if br == 2 and n - rel < 0:
                            continue
                        valid.append((ui, u, rel))
                stt_ = {}

                def stage_a():
                    sb2 = [self.bank(), self.bank()]
                    E, Pb = scratch()
                    stt_["Pb"] = Pb
                    for h2 in range(2):
                        pr = slice(64 * h2, 64 * h2 + 64)
                        sbk, skey = sb2[h2]
                        for ui, u, rel in valid:
                            it = ui * 2 + rel
                            if br == 1:
                                gk = 4 * n + u - rel
                                kap = self.khist[pr, hp, gk * 128:(gk + 1) * 128]
                                qap = aq[pr, u * 128:(u + 1) * 128]
                                kkey = ("K", hp, gk // 4)
                            else:
                                tn = n - rel
                                kap = self.khist[pr, hp, tn * T + u:(tn + 1) * T:4]
                                qap = aq[pr, u:T:4]
                                kkey = ("K", hp, tn)
                            self.mm(sbk[:, it * 128:(it + 1) * 128], kap, qap, True, True, [kkey] + aqk, [skey])
                        eb, ek = E[h2]
                        pt, pk = Pb[h2]
                        if len(valid) == 4:
                            self.act(eb[:, :], sbk[:, :], AF.Exp, [skey], [ek], scale=0.125)
                        else:
                            self.memset("pool", eb[:, :], 0.0, [ek], [ek])
                            for ui, u, rel in valid:
                                it = ui * 2 + rel
                                self.act(eb[:, it * 128:(it + 1) * 128], sbk[:, it * 128:(it + 1) * 128], AF.Exp,
                                         [skey, ek], [ek], scale=0.125)
                        self.tt("dve", pt[:, :].rearrange("p (u x) -> p u x", u=2),
                                eb[:, :].rearrange("p (u x) -> p u x", u=2),
                                dtab[:, hp, h2].rearrange("p a q -> p (a q)").unsqueeze(1).to_broadcast([128, 2, 256]),
                                ALU.mult, [ek, "const"], [pk])

                def stage_b():
                    Pb = stt_["Pb"]
                    for h2 in range(2):
                        pt, pk = Pb[h2]
                        for ui, u, rel in valid:
                            it = ui * 2 + rel
                            if br == 1:
                                slot = u - rel if u - rel >= 0 else 4
                                pv(self.v1[:, slot, hcol(h2)], pt, pk, slice(it * 128, (it + 1) * 128),
                                   slice(u * 128, (u + 1) * 128), 128, h2, [("V1", slot)])
                            else:
                                vpar = (n - rel) % 2
                                pv(self.v4[:, vpar, u, hcol(h2)], pt, pk, slice(it * 128, (it + 1) * 128),
                                   slice(u, T, 4), 128, h2, [("V4", vpar)])
                return stage_a, stage_b

            def make_unit3():
                nk = 32 * (n + 1)
                stt_ = {}

                def stage_a():
                    sb2 = [self.bank(), self.bank()]
                    E, Pb = scratch()
                    stt_["Pb"] = Pb
                    kk3 = [("K", hp, t_) for t_ in range(n + 1)]
                    for h2 in range(2):
                        pr = slice(64 * h2, 64 * h2 + 64)
                        sbk, skey = sb2[h2]
                        for r in range(16):
                            self.mm(sbk[0:nk, r * 32:(r + 1) * 32], self.khist[pr, hp, r:(n + 1) * T:16],
                                    aq[pr, r:T:16], True, True, kk3 + aqk, [skey])
                        eb, ek = E[h2]
                        pt, pk = Pb[h2]
                        self.act(eb[0:nk, :], sbk[0:nk, :], AF.Exp, [skey], [ek], scale=0.125)
                        self.tt("dve", pt[0:nk, :].rearrange("p (r q) -> p r q", r=16),
                                eb[0:nk, :].rearrange("p (r q) -> p r q", r=16),
                                C["d3"][0:nk, hp, h2, 32 * n:32 * n + 32].unsqueeze(1).to_broadcast([nk, 16, 32]),
                                ALU.mult, [ek, "const"], [pk])

                def stage_b():
                    Pb = stt_["Pb"]
                    v3k = [("V3", t_) for t_ in range(n + 1)]
                    for h2 in range(2):
                        pt, pk = Pb[h2]
                        for r in range(16):
                            pv(self.v3[0:nk, r, hcol(h2)], pt, pk, slice(r * 32, (r + 1) * 32), slice(r, T, 16), nk, h2, v3k,
                               last=(r == 15))
                return stage_a, stage_b

            for br in (1, 2):
                for pair in range(2):
                    units.append(make_unit12(br, pair))
            units.append(make_unit3())
            units[0][0]()
            for k in range(len(units)):
                if k + 1 < len(units):
                    units[k + 1][0]()
                    yield
                units[k][1]()
                yield
            rl = self.fs[4]
            self.recip_act(rl[:, :], lb[:, :], [lkey], [("fs", 4)])
            self.tt("dve", o32[hp][:, :], ob[:, :], rl[:, :], ALU.mult, [okey, ("fs", 4)], [("fs", hp)])
            self.release(okey, lkey)
            yield
        self.cp("pool", self.v1[:, 4, :], self.v1[:, 3, :], [("V1", 3)], [("V1", 4)])
        bk, bkey = self.bank()
        for hp in range(4):
            sq, sk = self.bs[hp % 2], ("bs", hp % 2)
            self.act(sq[:, :], o32[hp][:, :], AF.Square, [("fs", hp)], [sk])
            self.mm(bk[:, :], C["ones"][:], sq[:, :], hp == 0, hp == 3, ["const", sk], [bkey])
        rs = self.fs[4]
        self.rsqrt_act(rs[:, :], bk[:, :], 1.0 / 512, [bkey], [("fs", 4)])
        for hp in range(4):
            self.stt(self.yT[:, 4 + hp, :], o32[hp][:, :], self.vecs[:, 8 + hp:9 + hp], rs[:, :], ALU.mult, ALU.mult,
                     [("fs", hp), ("fs", 4), "vecs"], [("yT", 4 + hp)])

    def layer_norm(self, c, which):
        xk = ("xtok", c)
        xc = self.xtok[:, c, :]
        i = self.lnrot % 2
        self.lnrot += 1
        st = self.bnst[:, i]
        sk, lk = ("bnst", i), ("lns", i)
        for j in range(2):
            self.p.add("dve", (lambda j=j: (lambda e: e.bn_stats(out=st[:, j, :], in_=xc[:, j * 512:(j + 1) * 512])))(),
                       [xk], [sk])
        lns = self.lns[:, i, :]
        mv = lns[:, 0:2]
        self.p.add("dve", lambda e: e.bn_aggr(out=mv, in_=st.rearrange("p a b -> p (a b)")), [sk], [lk])
        rstd = lns[:, 2:3]
        self.rsqrt_act(rstd, lns[:, 1:2], 1.0, [lk], [lk])
        self.stt(xc, xc, lns[:, 0:1], self.lnp[:, 0, :], ALU.subtract, ALU.mult, [xk, lk, ("lnp", which)], [xk])
        self.stt(xc, xc, rstd, self.lnp[:, 1, :], ALU.mult, ALU.add, [xk, lk, ("lnp", which)], [xk])

    def load_lnp(self, l, which):
        self.dma("sp", self.lnp[:, :, :], self.prm["lnp"][l, 2 * which:2 * which + 2].rearrange("a p d -> p a d"),
                 [], [("lnp", 0), ("lnp", 1)])

    def phase_outproj(self, l, s, n):
        self.load_lnp(l, 0)
        bufs = [self.wblock(6, look=2), self.wblock(7, look=1)]
        yk = [("yT", j) for j in range(8)]

        def x1T(c):
            self.cp("act", self.xb[:], self.xtok[:, c, :], [("xtok", c)], ["xb"])
            self.xT_chunk(c, self.xb, ["xb"])

        for c in range(4):
            cs = slice(c * 128, (c + 1) * 128)
            for h in range(2):
                buf, wkey = bufs[h]
                bk, bkey = self.bank()
                for kt in range(8):
                    self.mm(bk[:, :], self.yT[:, kt, cs], buf[:, kt, :], kt == 0, kt == 7, [wkey] + yk, [bkey])
                xs = self.xtok[:, c, h * 512:(h + 1) * 512]
                self.stt(xs, xs, ALPHA, bk[:, :], ALU.mult, ALU.add, [("xtok", c), bkey], [("xtok", c)])
            self.layer_norm(c, 0)
            if c >= 1:
                x1T(c - 1)
        x1T(3)

    def phase_mlp(self, l, s, n):
        gA = ["gA", "gB"]
        w1 = self.w["mlp_w1"][l]
        w2 = self.w["mlp_w2"][l]
        self.load_lnp(l, 1)
        xTk = [("xT", c) for c in range(4)]
        for b in range(8):
            buf, wkey = self.wblock(8 + b)
            for m in range(4):
                bk, bkey = self.bank()
                for kt in range(8):
                    self.mm(bk[:, :], buf[:, kt, m * 128:(m + 1) * 128], self.xT[:, kt, :], kt == 0, kt == 7,
                            [wkey] + xTk, [bkey])
                tmp, tk = self.bs[(4 * b + m) % 4], ("bs", (4 * b + m) % 4)
                self.act(tmp[:, :], bk[:, :], AF.Relu, [bkey], [tk])
                self.tt("dve", self.hidden[:, 4 * b + m, :], tmp[:, :], tmp[:, :], ALU.mult, [tk] + gA, [("hid", 4 * b + m)])
        for h in range(2):
            banks = [self.bank(hold=True) for _ in range(4)]
            for q in range(4):
                buf, wkey = self.wblock(16 + 4 * h + q)
                for c in range(4):
                    bk, bkey = banks[c]
                    for kt in range(8):
                        kk = q * 8 + kt
                        self.mm(bk[:, :], self.hidden[:, kk, c * 128:(c + 1) * 128], buf[:, kt, :], kk == 0, kk == 31,
                                [wkey, ("hid", kk)] + gA, [bkey])
            for c in range(4):
                bk, bkey = banks[c]
                xs = self.xtok[:, c, h * 512:(h + 1) * 512]
                self.stt(xs, xs, ALPHA, bk[:, :], ALU.mult, ALU.add, [("xtok", c), bkey], [("xtok", c)])
            self.release(*[k for _, k in banks])
        for c in range(4):
            self.layer_norm(c, 1)


_CACHE = {}


def _get_program(layers, nseq=NSEQ, ntiles=NT, taps=(), phases=None):
    key = (tuple(layers), nseq, ntiles, tuple(sorted(taps)), None if phases is None else tuple(sorted(phases)))
    if key not in _CACHE:
        b = Builder(layers, nseq, ntiles, taps, phases)
        b.epsb = b.sb("epsb", [128, 1], F32)
        b.memset("dve", b.epsb[:], EPS, [], ["epsb_k"])
        nc = b.build()
        _CACHE[key] = (nc, b)
    return _CACHE[key]


def make_in_maps(inputs, nseq=NSEQ, ncores=8):
    x = np.ascontiguousarray(inputs["x"], dtype=np.float32)
    consts = make_consts()
    prm = layout_params(inputs)
    shared = {}
    for name, _ in WEIGHT_SPECS:
        shared[name] = np.ascontiguousarray(inputs[name], dtype=np.float32)
    for k, v in prm.items():
        shared[k] = v
    for k, v in consts.items():
        shared["c_" + k] = v
    maps = []
    for c in range(ncores):
        m = dict(shared)
        m["x"] = x[c * nseq:(c + 1) * nseq].reshape(nseq * SEQ, D)
        maps.append(m)
    return maps


LAUNCH_PLAN = [[0, 1]]


def kernel(**inputs):
    inputs = {k: np.asarray(v) for k, v in inputs.items()}
    x = inputs["x"]
    cur = dict(inputs)
    for layers in LAUNCH_PLAN:
        nc, b = _get_program(layers)
        maps = make_in_maps(cur)
        res = run_bass_kernel_spmd(nc, maps, core_ids=list(range(8)))
        out = np.stack([r["out"].reshape(NSEQ, SEQ, D) for r in res.results], axis=0).reshape(16, SEQ, D)
        cur = dict(inputs)
        cur["x"] = out
    return out.astype(np.float32)
```
